# Optimizing a Trainium2 kernel written in Bass

```python
import math
import jax, jax.numpy as jnp
from jax import lax
import numpy as np

D_MODEL = 1024
BATCH = 4
SEQ = 4096
DEPTH = 2

GRID_W = 64
N_EVEN = (DEPTH + 1) // 2
N_ODD = DEPTH // 2
D_FF = 4 * D_MODEL
NORM_EPS = 1e-6

SSD_HEAD_DIM = 64
SSD_WIDTH = D_MODEL
SSD_HEADS = SSD_WIDTH // SSD_HEAD_DIM
SSD_GROUPS = 2
SSD_STATE = 128
SSD_CONV = 5
SSD_CHUNK = 128
SSD_CONV_CH = SSD_WIDTH + 2 * SSD_GROUPS * SSD_STATE

NA_HEAD_DIM = 64
NA_WIDTH = D_MODEL
NA_HEADS = NA_WIDTH // NA_HEAD_DIM
NA_KH_MAX = 8
NA_KW = 16

AB_IN = SSD_WIDTH + SSD_CONV_CH + 2 * SSD_HEADS + 3 * NA_WIDTH
AB_MIX = SSD_WIDTH + NA_WIDTH

ML_HEADS = 8
ML_DV = 2 * D_MODEL // ML_HEADS
ML_DK = ML_DV // 2
ML_CHUNK = 128
ML_WIDTH = ML_HEADS * ML_DV
ML_QK = ML_HEADS * ML_DK
ML_IN = 2 * ML_QK + 2 * ML_WIDTH + 4 * ML_HEADS

kernel_name = "hybrid_ssd_natten_mlstm_encoder"


def rmsnorm(x, g):
    xf = x.astype(jnp.float32)
    y = xf * lax.rsqrt(jnp.mean(xf * xf, axis=-1, keepdims=True) + NORM_EPS)
    return (y * g.astype(jnp.float32)).astype(x.dtype)


def modulate(h, shift, scale):
    return h * (1 + scale[:, None, :]) + shift[:, None, :]


def centred_dwconv(u, w, b):
    pad = w.shape[0] // 2
    out = lax.conv_general_dilated(u, w[:, None, :].astype(u.dtype), window_strides=(1,),
                                   padding=[(pad, pad)], dimension_numbers=('NWC', 'WIO', 'NWC'),
                                   feature_group_count=u.shape[-1])
    return out + b


def segsum(a):
    T = a.shape[-1]
    cs = jnp.cumsum(a, axis=-1)
    diff = cs[..., :, None] - cs[..., None, :]
    mask = jnp.tril(jnp.ones((T, T), dtype=bool))
    return jnp.where(mask, diff, -jnp.inf)


def ssd_scan(xh, dt, a, bm, cm):
    Bsz, L, H, P = xh.shape
    G, N = bm.shape[-2:]
    R = H // G
    T = SSD_CHUNK
    nc = L // T
    X = (xh * dt[..., None]).reshape(Bsz, nc, T, G, R, P)
    adt = (dt * a).reshape(Bsz, nc, T, G, R).transpose(0, 3, 4, 1, 2)
    Bc = bm.reshape(Bsz, nc, T, G, N)
    Cc = cm.reshape(Bsz, nc, T, G, N)
    a_cs = jnp.cumsum(adt, axis=-1)
    Lmat = jnp.exp(segsum(adt))
    cb = jnp.einsum('bctgn,bcsgn->bgcts', Cc, Bc)
    y_diag = jnp.einsum('bgrcts,bcsgrp->bctgrp', cb[:, :, None] * Lmat, X)
    decay_states = jnp.exp(a_cs[..., -1:] - a_cs)
    states = jnp.einsum('bctgn,bgrct,bctgrp->bcgrpn', Bc, decay_states, X)
    states = jnp.concatenate([jnp.zeros_like(states[:, :1]), states], axis=1)
    chunk_decay = jnp.exp(segsum(jnp.pad(a_cs[..., -1], ((0, 0), (0, 0), (0, 0), (1, 0)))))
    new_states = jnp.einsum('bgrzc,bcgrpn->bzgrpn', chunk_decay, states)
    states = new_states[:, :-1]
    y_off = jnp.einsum('bctgn,bcgrpn,bgrct->bctgrp', Cc, states, jnp.exp(a_cs))
    return (y_diag + y_off).reshape(Bsz, L, H, P)


def ssd_branch(z, xbc, dt_raw, conv_w, conv_b, dt_bias, a_log, d_skip, norm_g):
    f32 = jnp.float32
    xbc = jax.nn.silu(centred_dwconv(xbc, conv_w, conv_b)).astype(f32)
    xs, bm, cm = jnp.split(xbc, [SSD_WIDTH, SSD_WIDTH + SSD_GROUPS * SSD_STATE], axis=-1)
    Bsz, L, _ = xs.shape
    xh = xs.reshape(Bsz, L, SSD_HEADS, SSD_HEAD_DIM)
    bm = bm.reshape(Bsz, L, SSD_GROUPS, SSD_STATE)
    cm = cm.reshape(Bsz, L, SSD_GROUPS, SSD_STATE)
    dt = jax.nn.softplus(dt_raw.astype(f32).reshape(Bsz, L, 2, SSD_HEADS) + dt_bias.astype(f32))
    a = -jnp.exp(a_log.astype(f32))
    y_f = ssd_scan(xh, dt[:, :, 0], a[0], bm, cm)
    fl = lambda t: jnp.flip(t, axis=1)
    y_b = fl(ssd_scan(fl(xh), fl(dt[:, :, 1]), a[1], fl(bm), fl(cm)))
    y = y_f + y_b + d_skip.astype(f32)[:, None] * xh
    y = y.reshape(Bsz, L, SSD_WIDTH)
    return rmsnorm(y * jax.nn.silu(z.astype(f32)), norm_g)


def na_branch(qkv, rpb):
    Bsz, L, _ = qkv.shape
    rows = L // GRID_W
    kh = min(NA_KH_MAX, rows)
    q, k, v = jnp.split(qkv.astype(jnp.float32), 3, axis=-1)
    grid = lambda t: t.reshape(Bsz, rows, GRID_W, NA_HEADS, NA_HEAD_DIM)
    q = grid(q) * (NA_HEAD_DIM ** -0.5)
    k = grid(k)
    v = grid(v)
    r_idx = np.arange(rows)
    row_start = np.clip(r_idx - kh // 2, 0, rows - kh)
    c_idx = np.arange(GRID_W)
    col_start = np.clip(c_idx - NA_KW // 2, 0, GRID_W - NA_KW)
    col_keys = col_start[:, None] + np.arange(NA_KW)[None, :]
    row_off = row_start[:, None] + np.arange(kh)[None, :] - r_idx[:, None] + NA_KH_MAX - 1
    col_off = col_keys - c_idx[:, None] + NA_KW - 1
    rpb = rpb.astype(jnp.float32)

    def one_row(args):
        q_r, rs, ro = args
        k_band = lax.dynamic_slice_in_dim(k, rs, kh, axis=1)
        v_band = lax.dynamic_slice_in_dim(v, rs, kh, axis=1)
        k_win = jnp.take(k_band, col_keys, axis=2)
        v_win = jnp.take(v_band, col_keys, axis=2)
        bias = rpb[:, ro[:, None, None], col_off[None, :, :]]
        s = jnp.einsum('bwhd,biwjhd->bhwij', q_r, k_win) + bias.transpose(0, 2, 1, 3)[None]
        p = jax.nn.softmax(s.reshape(Bsz, NA_HEADS, GRID_W, kh * NA_KW), axis=-1).reshape(s.shape)
        return jnp.einsum('bhwij,biwjhd->bwhd', p, v_win)

    out = lax.map(one_row, (q.transpose(1, 0, 2, 3, 4),
                            jnp.asarray(row_start, jnp.int32), jnp.asarray(row_off, jnp.int32)))
    return out.transpose(1, 0, 2, 3, 4).reshape(Bsz, L, NA_WIDTH)


def mlstm_chunkwise(q, k, v, i_pre, f_pre):
    Bsz, H, L, DK = q.shape
    DV = v.shape[-1]
    T = ML_CHUNK
    nc = L // T
    q = q.reshape(Bsz, H, nc, T, DK)
    k = k.reshape(Bsz, H, nc, T, DK)
    v = v.reshape(Bsz, H, nc, T, DV)
    ig = i_pre.reshape(Bsz, H, nc, T)
    b = jnp.cumsum(jax.nn.log_sigmoid(f_pre).reshape(Bsz, H, nc, T), axis=-1)
    g = b[..., -1]
    a = g[..., None] - b + ig
    m_loc = jnp.max(a, axis=-1)
    w = jnp.exp(a - m_loc[..., None])
    S_loc = jnp.einsum('bhct,bhctk,bhctv->bhckv', w, k, v)
    n_loc = jnp.einsum('bhct,bhctk->bhck', w, k)

    def step(carry, inp):
        C, n, m = carry
        S_c, n_c, m_c, g_c = inp
        m_new = jnp.maximum(g_c + m, m_c)
        s_old = jnp.exp(g_c + m - m_new)
        s_new = jnp.exp(m_c - m_new)
        C_new = s_old[..., None, None] * C + s_new[..., None, None] * S_c
        n_new = s_old[..., None] * n + s_new[..., None] * n_c
        return (C_new, n_new, m_new), (C, n, m)

    init = (jnp.zeros((Bsz, H, DK, DV), jnp.float32), jnp.zeros((Bsz, H, DK), jnp.float32),
            jnp.zeros((Bsz, H), jnp.float32))
    mv = lambda t: jnp.moveaxis(t, 2, 0)
    _, (C_prev, n_prev, m_prev) = lax.scan(step, init, (mv(S_loc), mv(n_loc), mv(m_loc), mv(g)))
    C_prev = jnp.moveaxis(C_prev, 0, 2)
    n_prev = jnp.moveaxis(n_prev, 0, 2)
    m_prev = jnp.moveaxis(m_prev, 0, 2)
    mask = jnp.tril(jnp.ones((T, T), dtype=bool))
    Dm = jnp.where(mask, b[..., :, None] - b[..., None, :] + ig[..., None, :], -jnp.inf)
    m_inter = b + m_prev[..., None]
    m_t = jnp.maximum(m_inter, jnp.max(Dm, axis=-1))
    sc = jnp.einsum('bhctk,bhcsk->bhcts', q, k) * jnp.exp(Dm - m_t[..., None])
    inter_scale = jnp.exp(m_inter - m_t)
    num = jnp.einsum('bhcts,bhcsv->bhctv', sc, v) + inter_scale[..., None] * jnp.einsum('bhctk,bhckv->bhctv', q, C_prev)
    den = jnp.sum(sc, axis=-1) + inter_scale * jnp.einsum('bhctk,bhck->bhct', q, n_prev)
    h = num / jnp.maximum(jnp.abs(den), jnp.exp(-m_t))[..., None]
    return h.reshape(Bsz, H, L, DV)


def mlstm_branch(proj, gate_b, head_g):
    Bsz, L, _ = proj.shape
    q, k, v, o, gates = jnp.split(proj.astype(jnp.float32),
                                  [ML_QK, 2 * ML_QK, 2 * ML_QK + ML_WIDTH, 2 * ML_QK + 2 * ML_WIDTH], axis=-1)
    heads = lambda t, d: t.reshape(Bsz, L, ML_HEADS, d).transpose(0, 2, 1, 3)
    q = heads(q, ML_DK) * (ML_DK ** -0.5)
    k = heads(k, ML_DK)
    v = heads(v, ML_DV)
    gates = (gates.reshape(Bsz, L, 4, ML_HEADS) + gate_b.astype(jnp.float32)).transpose(0, 2, 3, 1)
    h_f = mlstm_chunkwise(q, k, v, gates[:, 0], gates[:, 1])
    fl = lambda t: jnp.flip(t, axis=2)
    h_b = fl(mlstm_chunkwise(fl(q), fl(k), fl(v), fl(gates[:, 2]), fl(gates[:, 3])))
    h = (h_f + h_b).transpose(0, 2, 1, 3)
    h = rmsnorm(h, head_g.reshape(ML_HEADS, ML_DV))
    return jax.nn.sigmoid(o) * h.reshape(Bsz, L, ML_WIDTH)


def ssd_na_mixer(h, w_in, conv_w, conv_b, dt_bias, a_log, d_skip, ssd_norm, rpb, w_out):
    proj = h @ w_in
    s1 = SSD_WIDTH
    s2 = s1 + SSD_CONV_CH
    s3 = s2 + 2 * SSD_HEADS
    z, xbc, dt_raw, qkv = jnp.split(proj, [s1, s2, s3], axis=-1)
    y_ssd = ssd_branch(z, xbc, dt_raw, conv_w, conv_b, dt_bias, a_log, d_skip, ssd_norm)
    y_na = na_branch(qkv, rpb)
    y = jnp.concatenate([y_ssd.astype(jnp.float32), y_na], axis=-1)
    return y.astype(h.dtype) @ w_out


def mlstm_mixer(h, w_in, gate_b, head_g, w_out):
    y = mlstm_branch(h @ w_in, gate_b, head_g)
    return y.astype(h.dtype) @ w_out


def setup_inputs(seed: int = 0) -> dict:
    key = jax.random.key(seed)
    ks = jax.random.split(key, 20)
    nrm = lambda k, shape, s: jax.random.normal(k, shape, jnp.float32) * s
    x = nrm(ks[0], (BATCH, SEQ, D_MODEL), 1.0)
    c = nrm(ks[1], (BATCH, D_MODEL), 1.0)
    ada_w = nrm(ks[2], (DEPTH, D_MODEL, 6 * D_MODEL), 0.5 * D_MODEL ** -0.5)
    ada_b = nrm(ks[3], (DEPTH, 6 * D_MODEL), 0.02)
    norm_g = 1.0 + nrm(ks[4], (DEPTH, 4, D_MODEL), 0.02)
    mlp_w1 = nrm(ks[5], (DEPTH, D_MODEL, D_FF), D_MODEL ** -0.5)
    mlp_w2 = nrm(ks[6], (DEPTH, D_FF, D_MODEL), D_FF ** -0.5)
    ab_w_in = nrm(ks[7], (N_EVEN, D_MODEL, AB_IN), D_MODEL ** -0.5)
    ab_conv_w = nrm(ks[8], (N_EVEN, SSD_CONV, SSD_CONV_CH), SSD_CONV ** -0.5)
    ab_conv_b = nrm(ks[9], (N_EVEN, SSD_CONV_CH), 0.02)
    dt0 = jnp.exp(jax.random.uniform(ks[10], (N_EVEN, 2, SSD_HEADS), jnp.float32,
                                     minval=math.log(1e-3), maxval=math.log(1e-1)))
    ab_dt_bias = dt0 + jnp.log(-jnp.expm1(-dt0))
    ab_a_log = jnp.log(jax.random.uniform(ks[11], (N_EVEN, 2, SSD_HEADS), jnp.float32, minval=1.0, maxval=16.0))
    ab_d_skip = 1.0 + nrm(ks[12], (N_EVEN, SSD_HEADS), 0.02)
    ab_ssd_norm = 1.0 + nrm(ks[13], (N_EVEN, SSD_WIDTH), 0.02)
    ab_rpb = nrm(ks[14], (N_EVEN, NA_HEADS, 2 * NA_KH_MAX - 1, 2 * NA_KW - 1), 0.1)
    ab_w_out = nrm(ks[15], (N_EVEN, AB_MIX, D_MODEL), AB_MIX ** -0.5)
    ml_w_in = nrm(ks[16], (N_ODD, D_MODEL, ML_IN), D_MODEL ** -0.5)
    gate_base = jnp.array([0.0, 4.0, 0.0, 4.0], jnp.float32)[None, :, None]
    ml_gate_b = gate_base + nrm(ks[17], (N_ODD, 4, ML_HEADS), 0.1)
    ml_head_norm = 1.0 + nrm(ks[18], (N_ODD, ML_WIDTH), 0.02)
    ml_w_out = nrm(ks[19], (N_ODD, ML_WIDTH, D_MODEL), ML_WIDTH ** -0.5)
    return {"x": x, "c": c, "ada_w": ada_w, "ada_b": ada_b, "norm_g": norm_g,
            "mlp_w1": mlp_w1, "mlp_w2": mlp_w2,
            "ab_w_in": ab_w_in, "ab_conv_w": ab_conv_w, "ab_conv_b": ab_conv_b,
            "ab_dt_bias": ab_dt_bias, "ab_a_log": ab_a_log, "ab_d_skip": ab_d_skip,
            "ab_ssd_norm": ab_ssd_norm, "ab_rpb": ab_rpb, "ab_w_out": ab_w_out,
            "ml_w_in": ml_w_in, "ml_gate_b": ml_gate_b, "ml_head_norm": ml_head_norm, "ml_w_out": ml_w_out}


def reference(x, c, ada_w, ada_b, norm_g, mlp_w1, mlp_w2,
              ab_w_in, ab_conv_w, ab_conv_b, ab_dt_bias, ab_a_log, ab_d_skip, ab_ssd_norm, ab_rpb, ab_w_out,
              ml_w_in, ml_gate_b, ml_head_norm, ml_w_out):
    cond = jax.nn.silu(c)
    for layer in range(DEPTH):
        mod = cond @ ada_w[layer] + ada_b[layer]
        sh1, sc1, g1, sh2, sc2, g2 = jnp.split(mod, 6, axis=-1)
        h = modulate(rmsnorm(x, norm_g[layer, 0]), sh1, sc1)
        if layer % 2 == 0:
            j = layer // 2
            mixed = ssd_na_mixer(h, ab_w_in[j], ab_conv_w[j], ab_conv_b[j], ab_dt_bias[j], ab_a_log[j],
                                 ab_d_skip[j], ab_ssd_norm[j], ab_rpb[j], ab_w_out[j])
        else:
            j = layer // 2
            mixed = mlstm_mixer(h, ml_w_in[j], ml_gate_b[j], ml_head_norm[j], ml_w_out[j])
        x = x + g1[:, None, :] * rmsnorm(mixed, norm_g[layer, 1])
        h = modulate(rmsnorm(x, norm_g[layer, 2]), sh2, sc2)
        u = jnp.square(jax.nn.relu(h @ mlp_w1[layer])) @ mlp_w2[layer]
        x = x + g2[:, None, :] * rmsnorm(u, norm_g[layer, 3])
    return x
```

```python
import contextlib
import numpy as np
import concourse.bass as bass
import concourse.mybir as mybir
from concourse.bass_utils import run_bass_kernel_spmd

F32, BF16 = mybir.dt.float32, mybir.dt.bfloat16
AF = mybir.ActivationFunctionType
ALU = mybir.AluOpType
AX = mybir.AxisListType
ENG = ('pe', 'dve', 'act', 'pool', 'sp')

D = 1024; NTOK = 2048; NCH = 16; NEXT = 2304; NCHX = 18; DFF = 4096
AB_IN = 5664; ML_IN = 6176
NEG = -30000.0
DEBUG_STAGE = None


def mk(m, *a, **kw):
    return lambda e: getattr(e, m)(*a, **kw)


class Sched:
    def __init__(self, nc, es):
        self.nc, self.es = nc, es
        self.q = {e: [] for e in ENG}
        self.sems, self.cnt = {}, {}
        for e in ENG:
            self._mk('e_' + e)
        self.seen = {e: {} for e in ENG}
        self.lastw, self.readers = {}, {}

    def _mk(self, name):
        self.sems[name] = self.es.enter_context(self.nc.semaphore(name))
        self.cnt[name] = 0

    def _collect(self, eng, r, w):
        deps = {}

        def add(tok):
            if tok is None:
                return
            s, v = tok
            if s == 'e_pe' and eng == 'pe':
                return
            if deps.get(s, 0) < v:
                deps[s] = v
        for k in r:
            add(self.lastw.get(k))
        for k in w:
            add(self.lastw.get(k))
            for s, v in self.readers.get(k, {}).items():
                add((s, v))
        waits = []
        for s, v in deps.items():
            if self.seen[eng].get(s, 0) < v:
                waits.append((s, v))
                self.seen[eng][s] = v
        return waits

    def _commit(self, tok, r, w):
        for k in r:
            d = self.readers.setdefault(k, {})
            if d.get(tok[0], 0) < tok[1]:
                d[tok[0]] = tok[1]
        for k in w:
            self.lastw[k] = tok
            self.readers[k] = {}

    def op(self, eng, fn, r=(), w=()):
        waits = self._collect(eng, r, w)
        s = 'e_' + eng
        self.cnt[s] += 1
        self.q[eng].append((waits, fn, s, 1))
        self._commit((s, self.cnt[s]), r, w)

    def dma(self, eng, fn, slot, r=(), w=(), inc=16):
        if slot not in self.sems:
            self._mk(slot)
        waits = self._collect(eng, r, w)
        self.cnt[slot] += inc
        self.q[eng].append((waits, fn, slot, inc))
        self._commit((slot, self.cnt[slot]), r, w)

    def barrier(self):
        for e in ENG:
            waits = []
            for s, c in self.cnt.items():
                if c > 0 and self.seen[e].get(s, 0) < c and not (s == 'e_pe' and e == 'pe'):
                    waits.append((s, c))
                    self.seen[e][s] = c
            self.q[e].append((waits, None, None, 0))

    def replay(self, block):
        def run(name):
            def f(e):
                for waits, fn, s, inc in self.q[name]:
                    for ws, wv in waits:
                        e.wait_ge(self.sems[ws], wv)
                    if fn is not None:
                        ins = fn(e)
                        if inc == 1 and not s.startswith('e_'):
                            ins.then_inc(self.sems[s])
                        else:
                            ins.then_inc(self.sems[s], inc)
            return f
        block.tensor(run('pe'))
        block.vector(run('dve'))
        block.scalar(run('act'))
        block.gpsimd(run('pool'))
        block.sync(run('sp'))


class WStream:
    NSLOT = 2
    SLOT = 6144

    def __init__(self, S, tiles, order, I):
        self.S, self.tiles, self.order, self.I = S, tiles, order, I
        self.rec = []
        self.issued = 0
        self.i = 0

    def _issue(self, idx):
        spec = (self.order if self.order is not None else self.rec)[idx]
        slot = idx % self.NSLOT
        t = self.tiles[slot]
        off = 0
        for (nm, ix, kt, n) in spec:
            src = self.I[nm][ix]
            dst = t[:, off:off + kt * n].rearrange("p (k n) -> p k n", k=kt)
            srcv = src.rearrange("(k p) n -> p k n", p=128)
            self.S.dma('pool', (mk('dma_start', out=dst, in_=srcv)),
                       'w%d' % slot, w=[('w', slot)])
            off += kt * n

    def get(self, spec, free_prev=True):
        idx = self.i
        self.i += 1
        self.rec.append(spec)
        if self.order is None:
            self._issue(idx)
            self.issued = idx + 1
        else:
            lim = idx + self.NSLOT if free_prev else idx + 1
            while self.issued < min(len(self.order), lim):
                self._issue(self.issued)
                self.issued += 1
        slot = idx % self.NSLOT
        t = self.tiles[slot]
        outs, off = [], 0
        for (nm, ix, kt, n) in spec:
            outs.append(t[:, off:off + kt * n].rearrange("p (k n) -> p k n", k=kt))
            off += kt * n
        assert off <= self.SLOT, off
        return outs, ('w', slot)


def build_program(stage=None):
    nc = bass.Bass("TRN2", target_bir_lowering=False)
    din = lambda name, shape: nc.dram_tensor(name, list(shape), F32, kind="ExternalInput").ap()
    I = {}
    I['x'] = din('x', [NEXT, D])
    I['cvec'] = din('cvec', [128, 8])
    I['ada_w'] = din('ada_w', [2, D, 6 * D])
    I['ada_b'] = din('ada_b', [2, 128, 6 * D])
    I['ngb'] = din('ngb', [2, 4, 128, D])
    I['mlp_w1'] = din('mlp_w1', [2, D, DFF])
    I['mlp_w2'] = din('mlp_w2', [2, DFF, D])
    I['ml_w'] = din('ml_w', [D, ML_IN])
    I['ml_gb'] = din('ml_gb', [128, 32])
    I['ml_hn'] = din('ml_hn', [128, 2048])
    I['ml_wo'] = din('ml_wo', [2048, D])
    I['sel'] = din('sel', [128, 2])
    I['ml_wg'] = din('ml_wg', [D, 32])
    I['ab_w'] = din('ab_w', [D, AB_IN])
    I['ab_wo'] = din('ab_wo', [2048, D])
    I['ab_wdt'] = din('ab_wdt', [D, 32])
    I['ab_dtb'] = din('ab_dtb', [128, 32])
    I['ab_alog'] = din('ab_alog', [128, 32])
    I['ab_dsk'] = din('ab_dsk', [128, 16])
    I['ab_sn'] = din('ab_sn', [128, 8])
    I['ab_cw'] = din('ab_cw', [128, 12, 5])
    I['ab_cb'] = din('ab_cb', [128, 12])
    I['ab_bias'] = din('ab_bias', [16, 3, 128, 640])
    out = nc.dram_tensor('out', [NTOK, D], F32, kind="ExternalOutput").ap()
    xs = nc.dram_tensor('xs', [NTOK, D], F32).ap()
    modv = nc.dram_tensor('modv', [2, 6, 128, D], F32).ap()
    cc_in = nc.dram_tensor('cc_in', [128, 258], F32).ap()
    cc_out = nc.dram_tensor('cc_out', [256, 258], F32).ap()
    ss_in = nc.dram_tensor('ss_in', [128, 512], F32).ap()
    ss_out = nc.dram_tensor('ss_out', [256, 512], F32).ap()
    sa_dram = nc.dram_tensor('sa_dram', [NCH, 128, 512], BF16).ap()

    order = None
    for pass_i in range(2):
        if pass_i == 1:
            pass
    order = _emit(None, I, out, xs, modv, (cc_in, ss_in, sa_dram), (cc_out, ss_out), None, dry=True, stage=stage)
    _emit(nc, I, out, xs, modv, (cc_in, ss_in, sa_dram), (cc_out, ss_out), order, dry=False, stage=stage)
    return nc


class _Dry:
    def __getattr__(self, k):
        return self
    def __call__(self, *a, **k):
        return self
    def __getitem__(self, k):
        return self
    def __enter__(self):
        return self
    def __exit__(self, *a):
        return False


def _emit(nc, I, out, xs, modv, cc_in, cc_out, order, dry, stage):
    if dry:
        nc = _Dry()
        I = {k: _Dry() for k in I}
        out = xs = modv = _Dry()
        cc_in = cc_out = (_Dry(), _Dry(), _Dry())
    with contextlib.ExitStack() as es:
        S = Sched(nc, es)
        ml_ccin, ml_ccout = cc_in[0], cc_out[0]
        ss_ccin, ss_ccout = cc_in[1], cc_out[1]
        sa_d = cc_in[2]
        uid = [0]

        def sb(name, shape, dt=F32, st=es):
            uid[0] += 1
            return st.enter_context(nc.sbuf_tensor('%s_%d' % (name, uid[0]), list(shape), dt))
        hT = sb('hT', [128, 8, NEXT], BF16)
        acc = sb('acc', [128, NCH, D], F32)
        wt = [sb('wt%d' % i, [128, WStream.SLOT], BF16) for i in range(WStream.NSLOT)]
        W = WStream(S, wt, order, I)
        ident_b = sb('ident_b', [128, 128], BF16)
        I_cst = None
        PSB = [es.enter_context(nc.psum_tensor('psb%d' % i, [128, 512], F32)) for i in range(5)]
        PS2 = es.enter_context(nc.psum_tensor('ps2', [128, 1024], F32))
        PST = es.enter_context(nc.psum_tensor('pst', [128, 1024], BF16))
        bank_rr = [0]

        def bank():
            i = bank_rr[0] % 4
            bank_rr[0] += 1
            return PSB[i], ('ps', i)

        if not dry:
            I_cst = nc.dram_tensor('cst_in', [128, 6, 128], F32, kind="ExternalInput").ap()
        else:
            I_cst = _Dry()
        cstall = sb('cstall', [128, 6, 128], F32)
        S.dma('sp', mk('dma_start', out=cstall[:], in_=I_cst), 'd_cst', w=['cst'])
        S.op('dve', mk('tensor_copy', out=ident_b[:], in_=cstall[:, 0, :]), r=['cst'], w=['identb'])
        identf = cstall[:, 0, :]
        tri = cstall[:, 1, :]
        triT = cstall[:, 2, :]
        mneg = cstall[:, 3, :]
        mnegT = cstall[:, 4, :]
        ones_f = cstall[:, 5, :]

        epsc = sb('epsc', [128, 1], F32)
        S.op('dve', mk('memset', epsc[:], 1e-6), w=['epsc'])
        onec = sb('onec', [128, 1], F32)
        S.op('dve', mk('memset', onec[:], 1.0), w=['onec'])

        def rms_stats(src_ap, rkeys, ph, tag):
            junk = ph['junk']; ssq = ph['ssq']; rstd = ph['rstd']
            S.op('dve', mk('memset', ssq[:], 0.0), w=['ssq'])
            S.op('act', mk('activation', out=junk[:], in_=src_ap, func=AF.Square, accum_out=ssq[:]),
                 r=list(rkeys) + ['ssq'], w=['junk', 'ssq'])
            S.op('act', mk('activation', out=rstd[:], in_=ssq[:], func=AF.Sqrt, bias=epsc[:], scale=1.0 / D),
                 r=['ssq', 'epsc'], w=['rstd'])
            S.op('dve', mk('reciprocal', out=rstd[:], in_=rstd[:]), r=['rstd'], w=['rstd'])
            return rstd

        def prologue(c, src_ap, rkeys, ph, A, Sv):
            rstd = rms_stats(src_ap, rkeys, ph, 'p')
            tmp = ph['tmp']; hn = ph['hn']
            S.op('dve', mk('scalar_tensor_tensor', out=tmp[:], in0=src_ap, scalar=rstd[:, 0:1], in1=A[:],
                                                         op0=ALU.mult, op1=ALU.mult),
                 r=list(rkeys) + ['rstd', 'vecA'], w=['tmp'])
            S.op('dve', mk('tensor_tensor', out=hn[:], in0=tmp[:], in1=Sv[:], op=ALU.add),
                 r=['tmp', 'vecS'], w=['hn'])
            for kt in range(8):
                S.op('pe', mk('transpose', out=PST[:, kt * 128:(kt + 1) * 128],
                                                        in_=hn[:, kt * 128:(kt + 1) * 128], identity=ident_b[:]),
                     r=['hn', 'identb'], w=['pst'])
            S.op('act', mk('copy', out=hT[:, :, c * 128:(c + 1) * 128],
                                         in_=PST[:].rearrange("p (k n) -> p k n", k=8)),
                 r=['pst'], w=[('hT', c)])

        def load_vec(dst, l, j, key):
            S.dma('sp', mk('dma_start', out=dst[:], in_=modv[l, j]), 'd_' + key, r=[('modv', l, j)], w=[key])

        def epilogue(l, which, src_x, src_key, dst, ph, nxt):
            G = ph['G']
            load_vec(G, l, 2 if which == 0 else 5, 'vecG')
            if nxt is not None:
                nl, nj = nxt
                load_vec(ph['A'], nl, nj[0], 'vecA')
                load_vec(ph['S'], nl, nj[1], 'vecS')
            for c in range(NCH):
                xb = ph['xb'][c % 2]
                xk = ('xb', c % 2)
                S.dma('sp', mk('dma_start', out=xb[:], in_=src_x[c * 128:(c + 1) * 128, :]),
                      'xl%d' % (c % 2), r=[(src_key, c)], w=[xk])
                rstd = rms_stats(acc[:, c, :], [('acc', c)], ph, 'e')
                tmp = ph['tmp']
                S.op('dve', mk('scalar_tensor_tensor', out=tmp[:], in0=acc[:, c, :], scalar=rstd[:, 0:1],
                                                                  in1=G[:], op0=ALU.mult, op1=ALU.mult),
                     r=[('acc', c), 'rstd', 'vecG'], w=['tmp'])
                S.op('dve', mk('tensor_tensor', out=xb[:], in0=xb[:], in1=tmp[:], op=ALU.add),
                     r=['tmp', xk], w=[xk])
                dkey = ('xs', c) if dst is xs else ('out', c)
                S.dma('sp', mk('dma_start', out=dst[c * 128:(c + 1) * 128, :], in_=xb[:]),
                      'xst%d' % (c % 2), r=[xk], w=[dkey])
                if nxt is not None:
                    prologue(c, xb[:], [xk], ph, ph['A'], ph['S'])

        def acc_mm(c, lhs_list, wtiles, wkey, first):
            for half in range(2):
                ps, pk = bank()
                n = len(lhs_list)
                for i, (lap, lk) in enumerate(lhs_list):
                    S.op('pe', mk('matmul',
                        ps[:, :], lhsT=lap, rhs=wtiles[i][:, half * 512:(half + 1) * 512],
                        start=(i == 0), stop=(i == n - 1)), r=list(lk) + [wkey], w=[pk])
                dst = acc[:, c, half * 512:(half + 1) * 512]
                if first:
                    S.op('dve', mk('tensor_copy', out=dst, in_=ps[:, :]), r=[pk], w=[('acc', c)])
                else:
                    S.op('dve', mk('tensor_tensor', out=dst, in0=ps[:, :], in1=dst, op=ALU.add),
                         r=[pk, ('acc', c)], w=[('acc', c)])

        def alloc_norm_bufs(ph_es):
            ph = {}
            ph['junk'] = sb('junk', [128, D], BF16, ph_es)
            ph['ssq'] = sb('ssq', [128, 1], F32, ph_es)
            ph['rstd'] = sb('rstd', [128, 1], F32, ph_es)
            ph['tmp'] = sb('tmp', [128, D], F32, ph_es)
            ph['hn'] = sb('hn', [128, D], BF16, ph_es)
            ph['A'] = sb('vA', [128, D], F32, ph_es)
            ph['S'] = sb('vS', [128, D], F32, ph_es)
            ph['G'] = sb('vG', [128, D], F32, ph_es)
            ph['xb'] = [sb('xb%d' % i, [128, D], F32, ph_es) for i in range(2)]
            return ph

        with contextlib.ExitStack() as pe0:
            cf = sb('cf', [128, 8], F32, pe0)
            cb = sb('cb', [128, 8], BF16, pe0)
            crep = sb('crep', [128, 8, 128], BF16, pe0)
            adab = sb('adab', [128, D], F32, pe0)
            ngt = sb('ngt', [128, D], F32, pe0)
            mv = [sb('mv%d' % i, [128, D], F32, pe0) for i in range(2)]
            S.dma('sp', mk('dma_start', out=cf[:], in_=I['cvec']), 'd_cf', w=['cf'])
            S.op('act', mk('activation', out=cb[:], in_=cf[:], func=AF.Silu), r=['cf'], w=['cb'])
            S.op('dve', mk('tensor_copy', out=crep[:], in_=cb[:].unsqueeze(2).to_broadcast([128, 8, 128])),
                 r=['cb'], w=['crep'])
            n_mod = 0
            for l in range(2):
                for j in range(6):
                    S.dma('sp', mk('dma_start', out=adab[:], in_=I['ada_b'][l, :, j * D:(j + 1) * D]),
                          'd_adab', w=['adab'])
                    gi = {1: 0, 2: 1, 4: 2, 5: 3}.get(j)
                    if gi is not None:
                        S.dma('sp', mk('dma_start', out=ngt[:], in_=I['ngb'][l, gi]), 'd_ngt', w=['ngt'])
                    m = mv[n_mod % 2]
                    mvk = ('mv', n_mod % 2)
                    n_mod += 1
                    for half in range(2):
                        (wa,), wk = W.get([('ada_w', (l, slice(None), slice(j * D + half * 512, j * D + (half + 1) * 512)), 8, 512)])
                        ps, pk = bank()
                        for kt in range(8):
                            S.op('pe', mk('matmul', ps[:, :], lhsT=crep[:, kt, :], rhs=wa[:, kt, :],
                                start=(kt == 0), stop=(kt == 7)), r=['crep', wk], w=[pk])
                        hs = slice(half * 512, (half + 1) * 512)
                        S.op('dve', mk('tensor_tensor', out=m[:, hs], in0=ps[:, :], in1=adab[:, hs],
                                                                                op=ALU.add), r=[pk, 'adab'], w=[mvk])
                    if j in (1, 4):
                        S.op('dve', mk('scalar_tensor_tensor', out=m[:], in0=m[:], scalar=1.0, in1=ngt[:],
                                                                          op0=ALU.add, op1=ALU.mult),
                             r=[mvk, 'ngt'], w=[mvk])
                    elif j in (2, 5):
                        S.op('dve', mk('tensor_tensor', out=m[:], in0=m[:], in1=ngt[:], op=ALU.mult),
                             r=[mvk, 'ngt'], w=[mvk])
                    S.dma('sp', mk('dma_start', out=modv[l, j], in_=m[:]), 'mvst%d' % mvk[1],
                          r=[mvk], w=[('modv', l, j)])
        S.barrier()

        with contextlib.ExitStack() as p1:
            ph = alloc_norm_bufs(p1)
            load_vec(ph['A'], 0, 1, 'vecA')
            load_vec(ph['S'], 0, 0, 'vecS')
            for c in range(NCHX):
                xb = ph['xb'][c % 2]
                xk = ('xb', c % 2)
                S.dma('sp', mk('dma_start', out=xb[:], in_=I['x'][c * 128:(c + 1) * 128, :]),
                      'xl%d' % (c % 2), w=[xk])
                prologue(c, xb[:], [xk], ph, ph['A'], ph['S'])
        S.barrier()

        def zero_acc():
            for c in range(NCH):
                S.op('dve', mk('memset', acc[:, c, :], 0.0), w=[('acc', c)])

        def mlp(l):
            with contextlib.ExitStack() as pm:
                uT = [sb('uT%d' % i, [128, 4, NTOK], BF16, pm) for i in range(2)]
                rl = [sb('rl%d' % i, [128, 512], F32, pm) for i in range(2)]
                hkeys = [('hT', c) for c in range(NCH)]
                for blk in range(8):
                    u = uT[blk % 2]
                    uk = ('uT', blk % 2)
                    (w1,), w1k = W.get([('mlp_w1', (l, slice(None), slice(blk * 512, (blk + 1) * 512)), 8, 512)])
                    nrl = 0
                    for ft in range(4):
                        for tb in range(4):
                            ps, pk = bank()
                            for kt in range(8):
                                S.op('pe', mk('matmul',
                                    ps[:, :], lhsT=w1[:, kt, ft * 128:(ft + 1) * 128],
                                    rhs=hT[:, kt, tb * 512:(tb + 1) * 512], start=(kt == 0), stop=(kt == 7)),
                                    r=hkeys[tb * 4:tb * 4 + 4] + [w1k], w=[pk])
                            r_ = rl[nrl % 2]
                            rk = ('rl', nrl % 2)
                            nrl += 1
                            S.op('act', mk('activation', out=r_[:], in_=ps[:, :], func=AF.Relu),
                                 r=[pk], w=[rk])
                            S.op('dve', mk('tensor_tensor',
                                out=u[:, ft, tb * 512:(tb + 1) * 512], in0=r_[:], in1=r_[:], op=ALU.mult),
                                r=[rk], w=[uk])
                    (w2,), w2k = W.get([('mlp_w2', (l, slice(blk * 512, (blk + 1) * 512), slice(None)), 4, D)])
                    for c in range(NCH):
                        acc_mm(c, [(u[:, ft, c * 128:(c + 1) * 128], [uk]) for ft in range(4)],
                               [w2[:, ft, :] for ft in range(4)], w2k, first=(blk == 0))
            S.barrier()

        def run_epilogue(l, which, src_x, src_key, dst, nxt):
            with contextlib.ExitStack() as pp:
                ph = alloc_norm_bufs(pp)
                epilogue(l, which, src_x, src_key, dst, ph, nxt)
            S.barrier()


        def mlstm():
            with contextlib.ExitStack() as pm:
                hk = [('hT', c) for c in range(NCH)]
                qT = sb('qT', [128, NTOK], BF16, pm)
                kT = sb('kT', [128, NTOK], BF16, pm)
                ktm = sb('ktm', [128, NCH, 128], BF16, pm)
                v1 = sb('v1', [128, NCH, 260], BF16, pm)
                CbA = sb('CbA', [128, NCH, 260], BF16, pm)
                CbB = sb('CbB', [128, NCH, 260], BF16, pm)
                mA = sb('mA', [128, NCH], F32, pm)
                mB = sb('mB', [128, NCH], F32, pm)
                Cn = sb('Cn', [128, 260], F32, pm)
                mcur = sb('mcur', [128, 1], F32, pm)
                gates = sb('gates', [128, NCH, 32], F32, pm)
                nl = sb('nl', [128, NCH, 16], F32, pm)
                nb = sb('nb', [128, NCH, 16], F32, pm)
                u = sb('u', [128, NCH, 16], F32, pm)
                ngr = sb('ngr', [128, NCH, 16], F32, pm)
                umx = sb('umx', [128, NCH, 16], F32, pm)
                cmx = sb('cmx', [128, NCH, 16], F32, pm)
                gb = sb('gb', [128, 32], F32, pm)
                hnw = sb('hnw', [128, 2048], F32, pm)
                sel = sb('sel', [128, 2], F32, pm)
                wg = sb('wg', [128, 8, 32], BF16, pm)
                diag = sb('diag', [128, 128], F32, pm)
                Ep = sb('Ep', [128, 128], F32, pm)
                t2 = sb('t2', [128, 128], F32, pm)
                DxT = sb('DxT', [128, 128], F32, pm)
                scT = sb('scT', [128, 128], BF16, pm)
                kw = sb('kw', [128, 128], BF16, pm)
                sm = sb('sm', [128, 16], F32, pm)
                t5 = sb('t5', [128, 260], F32, pm)
                nd = sb('nd', [128, 260], F32, pm)
                hsum = sb('hsum', [128, 256], F32, pm)
                so = sb('so', [128, 256], F32, pm)
                yb = sb('yb', [128, 256], BF16, pm)
                yT = sb('yT', [128, 2, 128], BF16, pm)
                junk2 = sb('junk2', [128, 256], BF16, pm)
                xch = sb('xch', [128, 2, 258], F32, pm)
                stx = sb('stx', [128, 258], F32, pm)
                P4, P5, P6 = PSB[4], PS2[:, 0:512], PS2[:, 512:1024]

                S.dma('sp', mk('dma_start', out=gb[:], in_=I['ml_gb']), 'd_gb', w=['gb'])
                S.dma('sp', mk('dma_start', out=hnw[:], in_=I['ml_hn']), 'd_hnw', w=['hnw'])
                S.dma('sp', mk('dma_start', out=sel[:], in_=I['sel']), 'd_sel', w=['sel'])
                S.dma('pool', mk('dma_start', out=wg[:], in_=I['ml_wg'].rearrange("(k p) n -> p k n", p=128)),
                      'wgl', w=['wg'])
                S.op('dve', mk('memset', v1[:, :, 256:260], 1.0), w=['v1'])
                for c in range(NCH):
                    for kt in range(8):
                        S.op('pe', mk('matmul', P4[:, 0:32], lhsT=hT[:, kt, c * 128:(c + 1) * 128],
                                                                  rhs=wg[:, kt, :], start=(kt == 0), stop=(kt == 7)),
                             r=[hk[c], 'wg'], w=[('p4', 0)])
                    S.op('dve', mk('tensor_tensor', out=gates[:, c, :], in0=P4[:, 0:32], in1=gb[:], op=ALU.add),
                         r=[('p4', 0), 'gb'], w=['gates'])
                for d in range(2):
                    S.op('act', mk('activation', out=nl[:, :, d * 8:(d + 1) * 8], in_=gates[:, :, 16 * d + 8:16 * d + 16],
                                                            func=AF.Exp, scale=-1.0), r=['gates'], w=['nl'])
                S.op('act', mk('activation', out=nl[:], in_=nl[:], func=AF.Ln, bias=onec[:], scale=1.0),
                     r=['nl', 'onec'], w=['nl'])
                for c in range(NCH):
                    S.op('pe', mk('matmul', P4[:, 0:8], lhsT=tri, rhs=nl[:, c, 0:8], start=True, stop=True),
                         r=['nl', 'cst'], w=[('p4', 0)])
                    S.op('pe', mk('matmul', P4[:, 8:16], lhsT=triT, rhs=nl[:, c, 8:16], start=True, stop=True),
                         r=['nl', 'cst'], w=[('p4', 0)])
                    S.op('pe', mk('matmul', P4[:, 16:32], lhsT=ones_f, rhs=nl[:, c, :], start=True, stop=True),
                         r=['nl', 'cst'], w=[('p4', 0)])
                    S.op('dve', mk('tensor_copy', out=nb[:, c, :], in_=P4[:, 0:16]), r=[('p4', 0)], w=['nb'])
                    S.op('dve', mk('tensor_copy', out=ngr[:, c, :], in_=P4[:, 16:32]), r=[('p4', 0)], w=['ngr'])
                    for d in range(2):
                        S.op('dve', mk('tensor_tensor', out=u[:, c, d * 8:(d + 1) * 8], in0=nb[:, c, d * 8:(d + 1) * 8],
                                                                        in1=gates[:, c, 16 * d:16 * d + 8], op=ALU.add),
                             r=['nb', 'gates'], w=['u'])
                for c in range(NCH):
                    for j in range(16):
                        d = j // 8
                        S.op('dve', mk('tensor_scalar_mul', out=diag[:], in0=identf, scalar1=u[:, c, j:j + 1]),
                             r=['u', 'cst'], w=['diag'])
                        S.op('pe', mk('matmul', P4[:, 0:128], lhsT=ones_f, rhs=diag[:], start=True, stop=True),
                             r=['diag', 'cst'], w=[('p4', 0)])
                        S.op('dve', mk('reduce_max', out=umx[:, c, j:j + 1], in_=P4[:, 0:128], axis=AX.X),
                             r=[('p4', 0)], w=['umx'])
                        msk = mnegT if d == 0 else mneg
                        S.op('dve', mk('tensor_tensor', out=Ep[:], in0=P4[:, 0:128], in1=msk, op=ALU.add),
                             r=[('p4', 0), 'cst'], w=['Ep'])
                        S.op('dve', mk('reduce_max', out=cmx[:, c, j:j + 1], in_=Ep[:], axis=AX.X),
                             r=['Ep'], w=['cmx'])

                def state_step(c, j, Cb, mS):
                    S.op('act', mk('copy', out=Cb[:, c, 0:257], in_=Cn[:, 0:257]), r=['Cn'], w=['Cb'])
                    S.op('dve', mk('tensor_copy', out=mS[:, c:c + 1], in_=mcur[:]), r=['mcur'], w=['mS'])
                    S.op('dve', mk('tensor_scalar_mul', out=sm[:, 0:1], in0=umx[:, c, j:j + 1], scalar1=-1.0), r=['umx'], w=['sm'])
                    S.op('act', mk('activation', out=sm[:, 6:7], in_=u[:, c, j:j + 1], func=AF.Exp, bias=sm[:, 0:1], scale=1.0),
                         r=['u', 'sm'], w=['sm'])
                    S.op('dve', mk('tensor_scalar_mul', out=kw[:], in0=ktm[:, c, :], scalar1=sm[:, 6:7]), r=['ktm', 'sm'], w=['kw'])
                    ps, pk = bank()
                    S.op('pe', mk('matmul', ps[:, 0:257], lhsT=kw[:], rhs=v1[:, c, 0:257], start=True, stop=True),
                         r=['kw', 'v1'], w=[pk])
                    S.op('dve', mk('tensor_tensor', out=sm[:, 1:2], in0=mcur[:], in1=umx[:, c, j:j + 1], op=ALU.max),
                         r=['mcur', 'umx', 'sm'], w=['sm'])
                    S.op('dve', mk('tensor_tensor', out=sm[:, 2:3], in0=mcur[:], in1=sm[:, 1:2], op=ALU.subtract),
                         r=['mcur', 'sm'], w=['sm'])
                    S.op('dve', mk('tensor_tensor', out=sm[:, 3:4], in0=umx[:, c, j:j + 1], in1=sm[:, 1:2], op=ALU.subtract),
                         r=['umx', 'sm'], w=['sm'])
                    S.op('act', mk('activation', out=sm[:, 4:6], in_=sm[:, 2:4], func=AF.Exp), r=['sm'], w=['sm'])
                    S.op('act', mk('mul', out=t5[:, 0:257], in_=ps[:, 0:257], mul=sm[:, 5:6]),
                         r=[pk, 'sm'], w=['t5'])
                    S.op('dve', mk('scalar_tensor_tensor', out=Cn[:, 0:257], in0=Cn[:, 0:257], scalar=sm[:, 4:5], in1=t5[:, 0:257],
                                                                 op0=ALU.mult, op1=ALU.add), r=['Cn', 'sm', 't5'], w=['Cn'])
                    S.op('dve', mk('tensor_tensor', out=mcur[:], in0=sm[:, 1:2], in1=ngr[:, c, j:j + 1], op=ALU.subtract),
                         r=['sm', 'ngr'], w=['mcur'])

                def out_dir(c, j, Cb, mS, first):
                    d = j // 8
                    S.op('dve', mk('tensor_tensor', out=sm[:, 8:9], in0=cmx[:, c, j:j + 1], in1=mS[:, c:c + 1], op=ALU.max),
                         r=['cmx', 'mS', 'sm'], w=['sm'])
                    S.op('dve', mk('tensor_tensor', out=sm[:, 10:11], in0=mS[:, c:c + 1], in1=sm[:, 8:9], op=ALU.subtract),
                         r=['mS', 'sm'], w=['sm'])
                    S.op('dve', mk('tensor_tensor', out=sm[:, 12:13], in0=nb[:, c, j:j + 1], in1=sm[:, 8:9], op=ALU.subtract),
                         r=['nb', 'sm'], w=['sm'])
                    S.op('act', mk('activation', out=sm[:, 11:12], in_=sm[:, 10:11], func=AF.Exp), r=['sm'], w=['sm'])
                    S.op('act', mk('activation', out=sm[:, 13:14], in_=sm[:, 12:13], func=AF.Exp), r=['sm'], w=['sm'])
                    S.op('dve', mk('tensor_scalar_mul', out=diag[:], in0=identf, scalar1=sm[:, 8:9]),
                         r=['sm', 'cst'], w=['diag'])
                    S.op('pe', mk('matmul', P4[:, 128:256], lhsT=ones_f, rhs=diag[:], start=True, stop=True),
                         r=['diag', 'cst'], w=[('p4', 1)])
                    msk = mneg if d == 0 else mnegT
                    S.op('dve', mk('tensor_tensor', out=t2[:], in0=msk, in1=P4[:, 128:256], op=ALU.subtract),
                         r=[('p4', 1), 'cst'], w=['t2'])
                    S.op('act', mk('activation', out=DxT[:], in_=t2[:], func=AF.Exp, bias=u[:, c, j:j + 1], scale=1.0),
                         r=['t2', 'u'], w=['DxT'])
                    S.op('dve', mk('tensor_tensor', out=scT[:], in0=P4[:, 256:384], in1=DxT[:], op=ALU.mult),
                         r=[('p4', 2), 'DxT'], w=['scT'])
                    S.op('pe', mk('matmul', P5[:, 0:257], lhsT=scT[:], rhs=v1[:, c, 0:257], start=True, stop=True),
                         r=['scT', 'v1'], w=[('p5', 0)])
                    S.op('pe', mk('matmul', P6[:, 0:257], lhsT=qT[:, c * 128:(c + 1) * 128], rhs=Cb[:, c, 0:257],
                                                  start=True, stop=True), r=['qT', 'Cb'], w=[('p6', 0)])
                    S.op('act', mk('mul', out=t5[:, 0:257], in_=P6[:, 0:257], mul=sm[:, 11:12]),
                         r=[('p6', 0), 'sm'], w=['t5'])
                    S.op('dve', mk('tensor_tensor', out=nd[:, 0:257], in0=P5[:, 0:257], in1=t5[:, 0:257], op=ALU.add),
                         r=[('p5', 0), 't5'], w=['nd'])
                    S.op('act', mk('activation', out=sm[:, 14:15], in_=nd[:, 256:257], func=AF.Abs),
                         r=['nd', 'sm'], w=['sm'])
                    S.op('dve', mk('tensor_tensor', out=sm[:, 14:15], in0=sm[:, 14:15], in1=sm[:, 13:14], op=ALU.max),
                         r=['sm'], w=['sm'])
                    S.op('dve', mk('reciprocal', out=sm[:, 14:15], in_=sm[:, 14:15]), r=['sm'], w=['sm'])
                    if first:
                        S.op('dve', mk('tensor_scalar_mul', out=hsum[:], in0=nd[:, 0:256], scalar1=sm[:, 14:15]), r=['nd', 'sm'], w=['hsum'])
                    else:
                        S.op('dve', mk('scalar_tensor_tensor', out=hsum[:], in0=nd[:, 0:256], scalar=sm[:, 14:15], in1=hsum[:],
                                                                     op0=ALU.mult, op1=ALU.add), r=['nd', 'sm', 'hsum'], w=['hsum'])

                for h in range(8):
                    (wq, wk_, wv, wo_in), wkey = W.get([
                        ('ml_w', (slice(None), slice(h * 128, (h + 1) * 128)), 8, 128),
                        ('ml_w', (slice(None), slice(1024 + h * 128, 1024 + (h + 1) * 128)), 8, 128),
                        ('ml_w', (slice(None), slice(2048 + h * 256, 2048 + (h + 1) * 256)), 8, 256),
                        ('ml_w', (slice(None), slice(4096 + h * 256, 4096 + (h + 1) * 256)), 8, 256)])
                    (wout,), wokey2 = W.get([('ml_wo', (slice(h * 256, (h + 1) * 256), slice(None)), 2, D)], free_prev=False)
                    for (wsrc, dstT, scl) in ((wq, qT, 128.0 ** -0.5), (wk_, kT, 1.0)):
                        for tb in range(4):
                            ps, pk = bank()
                            for kt in range(8):
                                S.op('pe', mk('matmul',
                                    ps[:, :], lhsT=wsrc[:, kt, :], rhs=hT[:, kt, tb * 512:(tb + 1) * 512],
                                    start=(kt == 0), stop=(kt == 7)), r=hk[tb * 4:tb * 4 + 4] + [wkey], w=[pk])
                            dk_ = 'qT' if scl != 1.0 else 'kT'
                            S.op('act', mk('mul', out=dstT[:, tb * 512:(tb + 1) * 512], in_=ps[:, :], mul=scl), r=[pk], w=[dk_])
                    for g8 in range(2):
                        for cc in range(8):
                            c = g8 * 8 + cc
                            S.op('pe', mk('transpose', out=PST[:, cc * 128:(cc + 1) * 128],
                                                                         in_=kT[:, c * 128:(c + 1) * 128], identity=ident_b[:]),
                                 r=['kT', 'identb'], w=['pst'])
                        S.op('act', mk('copy', out=ktm[:, g8 * 8:(g8 + 1) * 8, :],
                                                            in_=PST[:].rearrange("p (k n) -> p k n", k=8)), r=['pst'], w=['ktm'])
                    for c in range(NCH):
                        ps, pk = bank()
                        for kt in range(8):
                            S.op('pe', mk('matmul', ps[:, 0:256], lhsT=hT[:, kt, c * 128:(c + 1) * 128],
                                                                             rhs=wv[:, kt, :], start=(kt == 0), stop=(kt == 7)),
                                 r=[hk[c], wkey], w=[pk])
                        S.op('act', mk('copy', out=v1[:, c, 0:256], in_=ps[:, 0:256]), r=[pk], w=['v1'])
                    S.op('dve', mk('memset', Cn[:], 0.0), w=['Cn'])
                    S.op('dve', mk('memset', mcur[:], 0.0), w=['mcur'])
                    for c in range(NCH):
                        state_step(c, h, CbA, mA)
                    S.op('dve', mk('tensor_copy', out=stx[:, 0:257], in_=Cn[:, 0:257]), r=['Cn'], w=['stx'])
                    S.op('dve', mk('tensor_copy', out=stx[:, 257:258], in_=mcur[:]), r=['mcur', 'stx'], w=['stx'])
                    S.dma('sp', mk('dma_start', out=ml_ccin, in_=stx[:]), 'ccs', r=['stx'], w=['ccin'])
                    S.dma('pool', mk('collective_compute', "AllGather", ALU.bypass, replica_groups=[[0, 1], [2, 3], [4, 5], [6, 7]],
                                                                 ins=[ml_ccin], outs=[ml_ccout]), 'cc', r=['ccin'], w=['ccout'], inc=1)
                    S.dma('sp', mk('dma_start', out=xch[:], in_=ml_ccout.rearrange("(a p) n -> p a n", p=128)), 'ccl',
                          r=['ccout'], w=['xch'])
                    S.op('dve', mk('tensor_scalar_mul', out=stx[:], in0=xch[:, 0, :], scalar1=sel[:, 0:1]),
                         r=['xch', 'sel'], w=['stx'])
                    S.op('dve', mk('scalar_tensor_tensor', out=stx[:], in0=xch[:, 1, :], scalar=sel[:, 1:2], in1=stx[:],
                                                                 op0=ALU.mult, op1=ALU.add), r=['xch', 'sel', 'stx'], w=['stx'])
                    S.op('dve', mk('tensor_copy', out=Cn[:, 0:257], in_=stx[:, 0:257]), r=['stx'], w=['Cn'])
                    S.op('dve', mk('tensor_copy', out=mcur[:], in_=stx[:, 257:258]), r=['stx'], w=['mcur'])
                    for c in range(NCH - 1, -1, -1):
                        state_step(c, 8 + h, CbB, mB)
                    for c in range(NCH):
                        S.op('pe', mk('matmul', P4[:, 256:384], lhsT=kT[:, c * 128:(c + 1) * 128],
                                                           rhs=qT[:, c * 128:(c + 1) * 128], start=True, stop=True),
                             r=['kT', 'qT'], w=[('p4', 2)])
                        out_dir(c, h, CbA, mA, True)
                        out_dir(c, 8 + h, CbB, mB, False)
                        ps, pk = bank()
                        for kt in range(8):
                            S.op('pe', mk('matmul', ps[:, 0:256], lhsT=hT[:, kt, c * 128:(c + 1) * 128],
                                                                             rhs=wo_in[:, kt, :], start=(kt == 0), stop=(kt == 7)),
                                 r=[hk[c], wkey], w=[pk])
                        S.op('act', mk('activation', out=so[:], in_=ps[:, 0:256], func=AF.Sigmoid), r=[pk], w=['so'])
                        S.op('dve', mk('memset', sm[:, 15:16], 0.0), r=['sm'], w=['sm'])
                        S.op('act', mk('activation', out=junk2[:], in_=hsum[:], func=AF.Square, accum_out=sm[:, 15:16]),
                             r=['hsum', 'sm'], w=['junk2', 'sm'])
                        S.op('act', mk('activation', out=sm[:, 15:16], in_=sm[:, 15:16], func=AF.Sqrt, bias=epsc[:], scale=1.0 / 256),
                             r=['sm', 'epsc'], w=['sm'])
                        S.op('dve', mk('reciprocal', out=sm[:, 15:16], in_=sm[:, 15:16]), r=['sm'], w=['sm'])
                        S.op('dve', mk('scalar_tensor_tensor', out=hsum[:], in0=hsum[:], scalar=sm[:, 15:16],
                                                                          in1=hnw[:, h * 256:(h + 1) * 256], op0=ALU.mult, op1=ALU.mult),
                             r=['hsum', 'sm', 'hnw'], w=['hsum'])
                        S.op('dve', mk('tensor_tensor', out=yb[:], in0=hsum[:], in1=so[:], op=ALU.mult),
                             r=['hsum', 'so'], w=['yb'])
                        for t in range(2):
                            S.op('pe', mk('transpose', out=PST[:, t * 128:(t + 1) * 128], in_=yb[:, t * 128:(t + 1) * 128],
                                                                  identity=ident_b[:]), r=['yb', 'identb'], w=['pst'])
                        S.op('act', mk('copy', out=yT[:], in_=PST[:, 0:256].rearrange("p (k n) -> p k n", k=2)),
                             r=['pst'], w=['yT'])
                        acc_mm(c, [(yT[:, 0, :], ['yT']), (yT[:, 1, :], ['yT'])], [wout[:, 0, :], wout[:, 1, :]], wokey2, first=(h == 0))
            S.barrier()


        def ssd():
            hk = [('hT', c) for c in range(NCHX)]
            P4 = PSB[4]
            with contextlib.ExitStack() as pm:
                dtb = sb('dtb', [128, 32], F32, pm)
                alog = sb('alog', [128, 32], F32, pm)
                dsk = sb('dsk', [128, 16], F32, pm)
                snc = sb('snc', [128, 8], F32, pm)
                cw = sb('cw', [128, 12, 5], F32, pm)
                cbv = sb('cbv', [128, 12], F32, pm)
                sel = sb('sel0', [128, 2], F32, pm)
                wdt = sb('wdt', [128, 8, 32], BF16, pm)
                dt = sb('dt', [128, NCH, 32], F32, pm)
                cs = sb('cs', [128, NCH, 32], F32, pm)
                ncs = sb('ncs', [128, NCH, 32], F32, pm)
                etot = sb('etot', [128, NCH, 32], F32, pm)
                w2 = sb('w2', [128, NCH, 32], F32, pm)
                ssq2 = sb('ssq2', [128, 2, NCH], F32, pm)
                for (t_, nm) in ((dtb, 'ab_dtb'), (alog, 'ab_alog'), (dsk, 'ab_dsk'), (snc, 'ab_sn'), (cbv, 'ab_cb'), (sel, 'sel')):
                    S.dma('sp', mk('dma_start', out=t_[:], in_=I[nm]), 'd_' + nm, w=[nm])
                S.dma('sp', mk('dma_start', out=cw[:], in_=I['ab_cw']), 'd_cw', w=['cw'])
                S.dma('pool', mk('dma_start', out=wdt[:], in_=I['ab_wdt'].rearrange("(k p) n -> p k n", p=128)), 'wgl', w=['wdt'])
                S.op('dve', mk('memset', ssq2[:], 0.0), w=['ssq2'])
                S.op('act', mk('activation', out=alog[:], in_=alog[:], func=AF.Exp), r=['ab_alog'], w=['ab_alog'])
                S.op('dve', mk('tensor_scalar_mul', out=alog[:], in0=alog[:], scalar1=-1.0), r=['ab_alog'], w=['ab_alog'])
                for c in range(NCH):
                    for kt in range(8):
                        S.op('pe', mk('matmul', P4[:, 0:32], lhsT=hT[:, kt, c * 128:(c + 1) * 128], rhs=wdt[:, kt, :],
                                      start=(kt == 0), stop=(kt == 7)), r=[hk[c], 'wdt'], w=[('p4', 0)])
                    S.op('dve', mk('tensor_tensor', out=dt[:, c, :], in0=P4[:, 0:32], in1=dtb[:], op=ALU.add),
                         r=[('p4', 0), 'ab_dtb'], w=['dt'])
                S.op('act', mk('activation', out=dt[:], in_=dt[:], func=AF.Exp), r=['dt'], w=['dt'])
                S.op('act', mk('activation', out=dt[:], in_=dt[:], func=AF.Ln, bias=onec[:], scale=1.0), r=['dt', 'onec'], w=['dt'])
                adt, tot = etot, w2
                S.op('dve', mk('tensor_tensor', out=adt[:], in0=dt[:], in1=alog[:].unsqueeze(1).to_broadcast([128, NCH, 32]), op=ALU.mult),
                     r=['dt', 'ab_alog'], w=['etot'])
                for c in range(NCH):
                    S.op('pe', mk('matmul', P4[:, 0:16], lhsT=tri, rhs=adt[:, c, 0:16], start=True, stop=True), r=['etot', 'cst'], w=[('p4', 0)])
                    S.op('pe', mk('matmul', P4[:, 16:32], lhsT=triT, rhs=adt[:, c, 16:32], start=True, stop=True), r=['etot', 'cst'], w=[('p4', 0)])
                    S.op('pe', mk('matmul', P4[:, 32:64], lhsT=ones_f, rhs=adt[:, c, :], start=True, stop=True), r=['etot', 'cst'], w=[('p4', 0)])
                    S.op('dve', mk('tensor_copy', out=cs[:, c, :], in_=P4[:, 0:32]), r=[('p4', 0)], w=['cs'])
                    S.op('dve', mk('tensor_copy', out=tot[:, c, :], in_=P4[:, 32:64]), r=[('p4', 0)], w=['w2'])
                S.op('dve', mk('tensor_scalar_mul', out=ncs[:], in0=cs[:], scalar1=-1.0), r=['cs'], w=['ncs'])
                S.op('act', mk('activation', out=etot[:], in_=tot[:], func=AF.Exp), r=['w2', 'etot'], w=['etot'])
                S.op('dve', mk('tensor_tensor', out=w2[:], in0=tot[:], in1=cs[:], op=ALU.subtract), r=['w2', 'cs'], w=['w2'])
                S.op('act', mk('activation', out=w2[:], in_=w2[:], func=AF.Exp), r=['w2'], w=['w2'])
                S.op('dve', mk('tensor_tensor', out=w2[:], in0=w2[:], in1=dt[:], op=ALU.mult), r=['w2', 'dt'], w=['w2'])

                xtm = sb('xtm', [128, NCH, 512], BF16, pm)
                Btm = sb('Btm', [128, NCH, 128], BF16, pm)
                BT = sb('BT', [128, NTOK], BF16, pm)
                CT = sb('CT', [128, NTOK], BF16, pm)
                Sst = sb('Sst', [128, 512], F32, pm)
                Sbf = sb('Sbf', [128, 512], BF16, pm)
                Xs = sb('Xs', [128, 512], BF16, pm)

                for g in range(2):
                    (wx, wB, wC), wkey = W.get([
                        ('ab_w', (slice(None), slice(1024 + g * 512, 1024 + (g + 1) * 512)), 8, 512),
                        ('ab_w', (slice(None), slice(2048 + g * 128, 2048 + (g + 1) * 128)), 8, 128),
                        ('ab_w', (slice(None), slice(2304 + g * 128, 2304 + (g + 1) * 128)), 8, 128)])
                    with contextlib.ExitStack() as pa:
                        cin = sb('cin', [128, 1028], F32, pa)
                        co = sb('co', [128, 1024], F32, pa)
                        xTt = sb('xTt', [128, NTOK], BF16, pa)
                        for ti in range(6):
                            wsrc = wx[:, :, ti * 128:(ti + 1) * 128] if ti < 4 else (wB if ti == 4 else wC)
                            ctile = (g * 4 + ti) if ti < 4 else (8 + g if ti == 4 else 10 + g)
                            dstT = xTt if ti < 4 else (BT if ti == 4 else CT)
                            dkey = 'xTt' if ti < 4 else ('BT' if ti == 4 else 'CT')
                            for hf in range(2):
                                base = hf * 1024 - 2
                                if hf == 0:
                                    S.op('dve', mk('memset', cin[:, 0:2], 0.0), r=['cin'], w=['cin'])
                                    pieces = [(0, 512), (512, 1024), (1024, 1026)]
                                else:
                                    pieces = [(1022, 1534), (1534, 2046), (2046, 2050)]
                                for (n0, n1) in pieces:
                                    ps, pk = bank()
                                    for kt in range(8):
                                        S.op('pe', mk('matmul', ps[:, 0:n1 - n0], lhsT=wsrc[:, kt, :], rhs=hT[:, kt, n0:n1],
                                                      start=(kt == 0), stop=(kt == 7)), r=hk[n0 // 128:(n1 - 1) // 128 + 1] + [wkey], w=[pk])
                                    S.op('act', mk('copy', out=cin[:, n0 - base:n1 - base], in_=ps[:, 0:n1 - n0]), r=[pk], w=['cin'])
                                S.op('dve', mk('tensor_scalar_mul', out=co[:], in0=cin[:, 0:1024], scalar1=cw[:, ctile, 0:1]),
                                     r=['cin', 'cw'], w=['co'])
                                for k in range(1, 5):
                                    S.op('dve', mk('scalar_tensor_tensor', out=co[:], in0=cin[:, k:k + 1024], scalar=cw[:, ctile, k:k + 1], in1=co[:],
                                                   op0=ALU.mult, op1=ALU.add), r=['cin', 'cw', 'co'], w=['co'])
                                S.op('act', mk('activation', out=dstT[:, hf * 1024:(hf + 1) * 1024], in_=co[:], func=AF.Silu,
                                               bias=cbv[:, ctile:ctile + 1], scale=1.0), r=['co', 'ab_cb'], w=[dkey])
                            if ti <= 4:
                                for g8 in range(2):
                                    for cc in range(8):
                                        c = g8 * 8 + cc
                                        S.op('pe', mk('transpose', out=PST[:, cc * 128:(cc + 1) * 128], in_=dstT[:, c * 128:(c + 1) * 128],
                                                      identity=ident_b[:]), r=[dkey, 'identb'], w=['pst'])
                                    if ti < 4:
                                        S.op('act', mk('copy', out=xtm[:, g8 * 8:(g8 + 1) * 8, ti * 128:(ti + 1) * 128],
                                                       in_=PST[:].rearrange("p (k n) -> p k n", k=8)), r=['pst'], w=['xtm'])
                                    else:
                                        S.op('act', mk('copy', out=Btm[:, g8 * 8:(g8 + 1) * 8, :],
                                                       in_=PST[:].rearrange("p (k n) -> p k n", k=8)), r=['pst'], w=['Btm'])

                    S.barrier()
                    with contextlib.ExitStack() as pb:
                        Xw = sb('Xw', [128, 2, 512], BF16, pb)
                        dg = sb('dg', [128, 4, 128], F32, pb)
                        tq = sb('tq', [128, 4, 128], F32, pb)
                        ER = sb('ER', [128, 4, 128], F32, pb)
                        MT = sb('MT', [128, 2, 4, 128], BF16, pb)
                        CsT = sb('CsT', [128, 2, 4, 128], BF16, pb)
                        zt = sb('zt', [128, 512], F32, pb)
                        yg = sb('yg', [128, 512], F32, pb)
                        ygb = sb('ygb', [128, 512], BF16, pb)
                        ygT = sb('ygT', [128, 4, 128], BF16, pb)
                        junk3 = sb('junk3', [128, 512], BF16, pb)
                        sast = [sb('sast%d' % i_, [128, 512], BF16, pb) for i_ in range(2)]
                        sald = [sb('sald%d' % i_, [128, 512], BF16, pb) for i_ in range(2)]
                        def bc8(t3, c, d):
                            return t3[:, c, d * 16 + g * 8:d * 16 + g * 8 + 8].unsqueeze(2).to_broadcast([128, 8, 64])

                        def state_update(c, d):
                            S.op('dve', mk('tensor_tensor', out=Xs[:].rearrange("p (h q) -> p h q", h=8),
                                           in0=xtm[:, c, :].rearrange("p (h q) -> p h q", h=8), in1=bc8(w2, c, d), op=ALU.mult),
                                 r=['xtm', 'w2'], w=['Xs'])
                            ps, pk = bank()
                            S.op('pe', mk('matmul', ps[:, :], lhsT=Btm[:, c, :], rhs=Xs[:], start=True, stop=True), r=['Btm', 'Xs'], w=[pk])
                            S.op('dve', mk('tensor_tensor', out=Sst[:].rearrange("p (h q) -> p h q", h=8),
                                           in0=Sst[:].rearrange("p (h q) -> p h q", h=8), in1=bc8(etot, c, d), op=ALU.mult),
                                 r=['Sst', 'etot'], w=['Sst'])
                            S.op('dve', mk('tensor_tensor', out=Sst[:], in0=Sst[:], in1=ps[:, :], op=ALU.add), r=['Sst', pk], w=['Sst'])

                        S.op('dve', mk('memset', Sst[:], 0.0), w=['Sst'])
                        for c in range(NCH):
                            S.op('act', mk('copy', out=sast[c % 2][:], in_=Sst[:]), r=['Sst'], w=[('sast', c % 2)])
                            S.dma('sp', mk('dma_start', out=sa_d[c], in_=sast[c % 2][:]), 'sast%d' % (c % 2), r=[('sast', c % 2)], w=[('sad', c)])
                            state_update(c, 0)
                        S.dma('sp', mk('dma_start', out=ss_ccin, in_=Sst[:]), 'ccs', r=['Sst'], w=['ccin0'])
                        S.dma('pool', mk('collective_compute', "AllGather", ALU.bypass, replica_groups=[[0, 1], [2, 3], [4, 5], [6, 7]],
                                         ins=[ss_ccin], outs=[ss_ccout]), 'cc', r=['ccin0'], w=['ccout0'], inc=1)
                        S.dma('sp', mk('dma_start', out=yg[:], in_=ss_ccout[0:128, :]), 'ccl', r=['ccout0'], w=['yg'])
                        S.dma('sp', mk('dma_start', out=zt[:], in_=ss_ccout[128:256, :]), 'ccl2', r=['ccout0'], w=['zt'])
                        S.op('dve', mk('tensor_scalar_mul', out=Sst[:], in0=yg[:], scalar1=sel[:, 0:1]), r=['yg', 'sel'], w=['Sst'])
                        S.op('dve', mk('scalar_tensor_tensor', out=Sst[:], in0=zt[:], scalar=sel[:, 1:2], in1=Sst[:],
                                       op0=ALU.mult, op1=ALU.add), r=['zt', 'sel', 'Sst'], w=['Sst'])
                        (wz,), wzkey = W.get([('ab_w', (slice(None), slice(g * 512, (g + 1) * 512)), 8, 512)])
                        (wo4,), wokey = W.get([('ab_wo', (slice(g * 512, (g + 1) * 512), slice(None)), 4, D)], free_prev=False)
                        for c in range(NCH - 1, -1, -1):
                            S.op('act', mk('copy', out=Sbf[:], in_=Sst[:]), r=['Sst'], w=['Sbf'])
                            S.dma('sp', mk('dma_start', out=sald[c % 2][:], in_=sa_d[c]), 'sald%d' % (c % 2), r=[('sad', c)], w=[('sald', c % 2)])
                            S.op('pe', mk('matmul', P4[:, 384:512], lhsT=BT[:, c * 128:(c + 1) * 128], rhs=CT[:, c * 128:(c + 1) * 128],
                                          start=True, stop=True), r=['BT', 'CT'], w=[('p4', 3)])
                            for d in range(2):
                                S.op('dve', mk('tensor_tensor', out=Xw[:, d, :].rearrange("p (h q) -> p h q", h=8),
                                               in0=xtm[:, c, :].rearrange("p (h q) -> p h q", h=8), in1=bc8(dt, c, d), op=ALU.mult),
                                     r=['xtm', 'dt'], w=['Xw'])
                            for hq in range(2):
                                for d in range(2):
                                    col0 = d * 16 + g * 8 + hq * 4
                                    ps, pk = bank()
                                    for i in range(4):
                                        S.op('dve', mk('tensor_scalar_mul', out=dg[:, i, :], in0=identf, scalar1=cs[:, c, col0 + i:col0 + i + 1]),
                                             r=['cs', 'cst'], w=[('dg', i)])
                                        S.op('pe', mk('matmul', ps[:, i * 128:(i + 1) * 128], lhsT=ones_f, rhs=dg[:, i, :], start=True, stop=True),
                                             r=[('dg', i), 'cst'], w=[pk])
                                    msk = mneg if d == 0 else mnegT
                                    S.op('dve', mk('tensor_tensor', out=tq[:], in0=ps[:, :].rearrange("p (i t) -> p i t", i=4),
                                                   in1=msk.unsqueeze(1).to_broadcast([128, 4, 128]), op=ALU.add), r=[pk, 'cst'], w=['tq'])
                                    S.op('dve', mk('tensor_tensor', out=tq[:], in0=tq[:],
                                                   in1=ncs[:, c, col0:col0 + 4].unsqueeze(2).to_broadcast([128, 4, 128]), op=ALU.add),
                                         r=['tq', 'ncs'], w=['tq'])
                                    S.op('act', mk('activation', out=tq[:], in_=tq[:], func=AF.Exp), r=['tq'], w=['tq'])
                                    S.op('act', mk('activation', out=ER[:], in_=ps[:, :].rearrange("p (i t) -> p i t", i=4), func=AF.Exp),
                                         r=[pk], w=['ER'])
                                    S.op('dve', mk('tensor_tensor', out=MT[:, d, :, :], in0=tq[:],
                                                   in1=P4[:, 384:512].unsqueeze(1).to_broadcast([128, 4, 128]), op=ALU.mult),
                                         r=['tq', ('p4', 3)], w=[('MT', d)])
                                    S.op('dve', mk('tensor_tensor', out=CsT[:, d, :, :], in0=ER[:],
                                                   in1=CT[:, c * 128:(c + 1) * 128].unsqueeze(1).to_broadcast([128, 4, 128]), op=ALU.mult),
                                         r=['ER', 'CT'], w=[('CsT', d)])
                                psy, pky = bank()
                                for i in range(4):
                                    hh = hq * 4 + i
                                    ysl = psy[:, i * 64:(i + 1) * 64]
                                    S.op('pe', mk('matmul', ysl, lhsT=MT[:, 0, i, :], rhs=Xw[:, 0, hh * 64:(hh + 1) * 64], start=True, stop=False),
                                         r=[('MT', 0), 'Xw'], w=[pky])
                                    S.op('pe', mk('matmul', ysl, lhsT=MT[:, 1, i, :], rhs=Xw[:, 1, hh * 64:(hh + 1) * 64], start=False, stop=False),
                                         r=[('MT', 1), 'Xw'], w=[pky])
                                    S.op('pe', mk('matmul', ysl, lhsT=CsT[:, 0, i, :], rhs=sald[c % 2][:, hh * 64:(hh + 1) * 64], start=False, stop=False),
                                         r=[('CsT', 0), ('sald', c % 2)], w=[pky])
                                    S.op('pe', mk('matmul', ysl, lhsT=CsT[:, 1, i, :], rhs=Sbf[:, hh * 64:(hh + 1) * 64], start=False, stop=True),
                                         r=[('CsT', 1), 'Sbf'], w=[pky])
                                hs = slice(hq * 256, (hq + 1) * 256)
                                S.op('dve', mk('tensor_tensor', out=yg[:, hs].rearrange("p (h q) -> p h q", h=4),
                                               in0=xtm[:, c, hs].rearrange("p (h q) -> p h q", h=4),
                                               in1=dsk[:, g * 8 + hq * 4:g * 8 + hq * 4 + 4].unsqueeze(2).to_broadcast([128, 4, 64]), op=ALU.mult),
                                     r=['xtm', 'ab_dsk'], w=['yg'])
                                S.op('dve', mk('tensor_tensor', out=yg[:, hs], in0=yg[:, hs], in1=psy[:, 0:256], op=ALU.add),
                                     r=['yg', pky], w=['yg'])
                            ps, pk = bank()
                            for kt in range(8):
                                S.op('pe', mk('matmul', ps[:, :], lhsT=hT[:, kt, c * 128:(c + 1) * 128], rhs=wz[:, kt, :],
                                              start=(kt == 0), stop=(kt == 7)), r=[hk[c], wzkey], w=[pk])
                            S.op('act', mk('activation', out=zt[:], in_=ps[:, :], func=AF.Silu), r=[pk], w=['zt'])
                            S.op('dve', mk('tensor_tensor', out=yg[:], in0=yg[:], in1=zt[:], op=ALU.mult), r=['yg', 'zt'], w=['yg'])
                            S.op('act', mk('activation', out=junk3[:], in_=yg[:], func=AF.Square, accum_out=ssq2[:, g, c:c + 1]),
                                 r=['yg', 'ssq2'], w=['junk3', 'ssq2'])
                            S.op('dve', mk('tensor_copy', out=ygb[:], in_=yg[:]), r=['yg'], w=['ygb'])
                            for t in range(4):
                                S.op('pe', mk('transpose', out=PST[:, t * 128:(t + 1) * 128], in_=ygb[:, t * 128:(t + 1) * 128], identity=ident_b[:]),
                                     r=['ygb', 'identb'], w=['pst'])
                            for t in range(4):
                                S.op('act', mk('mul', out=ygT[:, t, :], in_=PST[:, t * 128:(t + 1) * 128], mul=snc[:, g * 4 + t:g * 4 + t + 1]),
                                     r=['pst', 'ab_sn'], w=['ygT'])
                            acc_mm(c, [(ygT[:, t, :], ['ygT']) for t in range(4)], [wo4[:, t, :] for t in range(4)], wokey, first=(g == 0))
                            state_update(c, 1)
                    S.barrier()
                rs = sb('rs', [128, NCH], F32, pm)
                S.op('dve', mk('tensor_tensor', out=rs[:], in0=ssq2[:, 0, :], in1=ssq2[:, 1, :], op=ALU.add), r=['ssq2'], w=['rs'])
                S.op('act', mk('activation', out=rs[:], in_=rs[:], func=AF.Sqrt, bias=epsc[:], scale=1.0 / 1024), r=['rs', 'epsc'], w=['rs'])
                S.op('dve', mk('reciprocal', out=rs[:], in_=rs[:]), r=['rs'], w=['rs'])
                for c in range(NCH):
                    S.op('dve', mk('tensor_scalar_mul', out=acc[:, c, :], in0=acc[:, c, :], scalar1=rs[:, c:c + 1]),
                         r=[('acc', c), 'rs'], w=[('acc', c)])
            S.barrier()

        def na():
            hk = [('hT', c) for c in range(NCHX)]
            with contextlib.ExitStack() as pm:
                qT = sb('nqT', [128, NTOK], BF16, pm)
                kT = sb('nkT', [128, NEXT], BF16, pm)
                vtm = sb('vtm', [128, NCHX, 2, 66], BF16, pm)
                bt = sb('bt', [128, 2, 3, 640], F32, pm)
                sbt = sb('sbt', [128, 640], F32, pm)
                PT = sb('PT', [128, 5, 128], BF16, pm)
                rsn = sb('rsn', [128, 1], F32, pm)
                yna = sb('yna', [128, 128], BF16, pm)
                ynT = sb('ynT', [128, 128], BF16, pm)
                S.op('dve', mk('memset', vtm[:, :, :, 64:66], 1.0), w=['vtm'])
                for j in range(8):
                    (wq, wk_, wv, wo1), wkey = W.get([
                        ('ab_w', (slice(None), slice(2592 + j * 128, 2592 + (j + 1) * 128)), 8, 128),
                        ('ab_w', (slice(None), slice(3616 + j * 128, 3616 + (j + 1) * 128)), 8, 128),
                        ('ab_w', (slice(None), slice(4640 + j * 128, 4640 + (j + 1) * 128)), 8, 128),
                        ('ab_wo', (slice(1024 + j * 128, 1024 + (j + 1) * 128), slice(None)), 1, D)])
                    S.dma('sp', mk('dma_start', out=bt[:], in_=I['ab_bias'][2 * j:2 * j + 2].rearrange("h v p n -> p h v n")),
                          'd_bt', w=['bt'])
                    for (wsrc, dstT, scl, ntb, dk_) in ((wq, qT, 0.125, 4, 'nqT'), (wk_, kT, 1.0, 5, 'nkT')):
                        for tb in range(ntb):
                            n0, n1 = tb * 512, min((tb + 1) * 512, NEXT)
                            ps, pk = bank()
                            for kt in range(8):
                                S.op('pe', mk('matmul', ps[:, 0:n1 - n0], lhsT=wsrc[:, kt, :], rhs=hT[:, kt, n0:n1],
                                              start=(kt == 0), stop=(kt == 7)), r=hk[tb * 4:min(tb * 4 + 4, NCHX)] + [wkey], w=[pk])
                            S.op('act', mk('mul', out=dstT[:, n0:n1], in_=ps[:, 0:n1 - n0], mul=scl), r=[pk], w=[dk_])
                    for c in range(NCHX):
                        ps, pk = bank()
                        for kt in range(8):
                            S.op('pe', mk('matmul', ps[:, 0:128], lhsT=hT[:, kt, c * 128:(c + 1) * 128], rhs=wv[:, kt, :],
                                          start=(kt == 0), stop=(kt == 7)), r=[hk[c], wkey], w=[pk])
                        S.op('act', mk('copy', out=vtm[:, c, :, 0:64], in_=ps[:, 0:128].rearrange("p (e q) -> p e q", e=2)),
                             r=[pk], w=['vtm'])
                    for m in range(NCH):
                        j0 = min(max(m - 2, 0), 13)
                        var = min(m, 2)
                        for e_ in range(2):
                            prt = slice(e_ * 64, (e_ + 1) * 64)
                            for kt in range(5):
                                S.op('pe', mk('matmul', PS2[:, kt * 128:(kt + 1) * 128], lhsT=kT[prt, (j0 + kt) * 128:(j0 + kt + 1) * 128],
                                              rhs=qT[prt, m * 128:(m + 1) * 128], start=True, stop=True), r=['nkT', 'nqT'], w=['ps2'])
                            S.op('dve', mk('tensor_tensor', out=sbt[:, 0:512], in0=PS2[:, 0:512], in1=bt[:, e_, var, 0:512], op=ALU.add),
                                 r=['ps2', 'bt'], w=['sbt'])
                            S.op('dve', mk('tensor_tensor', out=sbt[:, 512:640], in0=PS2[:, 512:640], in1=bt[:, e_, var, 512:640], op=ALU.add),
                                 r=['ps2', 'bt', 'sbt'], w=['sbt'])
                            S.op('act', mk('activation', out=PT[:].rearrange("p k n -> p (k n)"), in_=sbt[:], func=AF.Exp), r=['sbt'], w=['PT'])
                            ps, pk = bank()
                            for kt in range(5):
                                S.op('pe', mk('matmul', ps[:, 0:65], lhsT=PT[:, kt, :], rhs=vtm[:, j0 + kt, e_, 0:65],
                                              start=(kt == 0), stop=(kt == 4)), r=['PT', 'vtm'], w=[pk])
                            S.op('dve', mk('reciprocal', out=rsn[:], in_=ps[:, 64:65]), r=[pk], w=['rsn'])
                            S.op('dve', mk('tensor_scalar_mul', out=yna[:, prt], in0=ps[:, 0:64], scalar1=rsn[:, 0:1]),
                                 r=[pk, 'rsn'], w=['yna'])
                        S.op('pe', mk('transpose', out=PST[:, 0:128], in_=yna[:], identity=ident_b[:]), r=['yna', 'identb'], w=['pst'])
                        S.op('act', mk('copy', out=ynT[:], in_=PST[:, 0:128]), r=['pst'], w=['ynT'])
                        acc_mm(m, [(ynT[:], ['ynT'])], [wo1[:, 0, :]], wkey, first=False)
            S.barrier()

        def body():
            if stage in ('mlp_only', 'l1'):
                zero_acc()
            else:
                if stage == 'na':
                    zero_acc()
                else:
                    ssd()
                if stage != 'ssd':
                    na()
            if stage in ('ssd', 'na', 'l0'):
                run_epilogue(0, 0, I['x'], 'xin', out, None)
                return
            run_epilogue(0, 0, I['x'], 'xin', xs, (0, (4, 3)))
            mlp(0)
            run_epilogue(0, 1, xs, 'xs', xs, (1, (1, 0)))
            if stage == 'mlp_only':
                zero_acc()
            else:
                mlstm()
            run_epilogue(1, 0, xs, 'xs', xs, (1, (4, 3)))
            mlp(1)
            run_epilogue(1, 1, xs, 'xs', out, None)
        body()

        if dry:
            return W.rec
        with nc.Block() as block:
            S.replay(block)
    return None


def _consts():
    s = np.arange(128)[:, None]
    t = np.arange(128)[None, :]
    c = np.zeros((128, 6, 128), np.float32)
    c[:, 0] = (s == t)
    c[:, 1] = (s <= t)
    c[:, 2] = (s >= t)
    c[:, 3] = np.where(s > t, NEG, 0.0)
    c[:, 4] = np.where(s < t, NEG, 0.0)
    c[:, 5] = 1.0
    return c


def _na_bias_tables(rpb, flipped):
    out = np.full((16, 3, 128, 5, 128), NEG, np.float32)
    kp = np.arange(128); qp = np.arange(128)
    for var in range(3):
        m = var
        j0 = 0
        for kt in range(5):
            lkr = 2 * (j0 + kt) + kp // 64
            lkc = kp % 64
            lqr = 2 * m + qp // 64
            lqc = qp % 64
            if flipped:
                gkr, gkc, gqr, gqc = 63 - lkr, 63 - lkc, 63 - lqr, 63 - lqc
            else:
                gkr, gkc, gqr, gqc = lkr, lkc, lqr, lqc
            rs = np.clip(gqr - 4, 0, 56); cs_ = np.clip(gqc - 8, 0, 48)
            dr = gkr[:, None] - gqr[None, :]
            dc = gkc[:, None] - gqc[None, :]
            valid = ((gkr[:, None] >= rs[None, :]) & (gkr[:, None] < rs[None, :] + 8) &
                     (gkc[:, None] >= cs_[None, :]) & (gkc[:, None] < cs_[None, :] + 16) &
                     (gkr[:, None] >= 0) & (gkr[:, None] < 64))
            ro = np.clip(dr + 7, 0, 14); co = np.clip(dc + 15, 0, 30)
            vals = rpb[:, ro, co]
            out[:, var, :, kt, :] = np.where(valid[None], vals, NEG)
    return np.ascontiguousarray(out.reshape(16, 3, 128, 640))


def _prep_inputs(inp):
    f = lambda k: np.ascontiguousarray(np.asarray(inp[k], dtype=np.float32))
    x = f('x'); c = f('c')
    ada_w = f('ada_w'); ada_b = f('ada_b'); norm_g = f('norm_g')
    shared = {
        'ada_w': ada_w,
        'ada_b': np.ascontiguousarray(np.broadcast_to(ada_b[:, None, :], (2, 128, 6 * D))),
        'ngb': np.ascontiguousarray(np.broadcast_to(norm_g[:, :, None, :], (2, 4, 128, D))),
        'mlp_w1': f('mlp_w1'), 'mlp_w2': f('mlp_w2'),
        'ml_w': f('ml_w_in')[0], 'ml_wo': f('ml_w_out')[0],
        'ml_hn': np.ascontiguousarray(np.broadcast_to(f('ml_head_norm')[0][None, :], (128, 2048))),
        'cst_in': _consts(),
        'ab_w': f('ab_w_in')[0], 'ab_wo': f('ab_w_out')[0],
        'ab_dsk': np.ascontiguousarray(np.broadcast_to(f('ab_d_skip')[0][None, :], (128, 16))),
        'ab_sn': np.ascontiguousarray(f('ab_ssd_norm')[0].reshape(8, 128).T),
        'ab_cb': np.ascontiguousarray(f('ab_conv_b')[0].reshape(12, 128).T),
    }
    rpb = f('ab_rpb')[0]
    btab = [_na_bias_tables(rpb, False), _na_bias_tables(rpb, True)]
    maps = []
    for core in range(8):
        b, s = core // 2, core % 2
        xl = x[b] if s == 0 else x[b][::-1]
        m = dict(shared)
        m['x'] = np.ascontiguousarray(xl[:NEXT])
        m['cvec'] = np.ascontiguousarray(c[b].reshape(8, 128).T)
        gb = f('ml_gate_b')[0]
        if s == 1:
            gb = gb[[2, 3, 0, 1]]
        m['ml_gb'] = np.ascontiguousarray(np.broadcast_to(gb.reshape(1, 32), (128, 32)))
        wgc = f('ml_w_in')[0][:, 6144:6176].reshape(D, 4, 8)
        if s == 1:
            wgc = wgc[:, [2, 3, 0, 1]]
        m['ml_wg'] = np.ascontiguousarray(wgc.reshape(D, 32))
        wdt = f('ab_w_in')[0][:, 2560:2592].reshape(D, 2, 16)
        dtb = f('ab_dt_bias')[0]; alog = f('ab_a_log')[0]; cwv = f('ab_conv_w')[0]
        if s == 1:
            wdt = wdt[:, ::-1]; dtb = dtb[::-1]; alog = alog[::-1]; cwv = cwv[::-1]
        m['ab_wdt'] = np.ascontiguousarray(wdt.reshape(D, 32))
        m['ab_dtb'] = np.ascontiguousarray(np.broadcast_to(dtb.reshape(1, 32), (128, 32)))
        m['ab_alog'] = np.ascontiguousarray(np.broadcast_to(alog.reshape(1, 32), (128, 32)))
        m['ab_cw'] = np.ascontiguousarray(cwv.T.reshape(12, 128, 5).transpose(1, 0, 2))
        m['ab_bias'] = btab[s]
        sel = np.zeros((128, 2), np.float32)
        sel[:, 1 - s] = 1.0
        m['sel'] = sel
        maps.append(m)
    return maps


_NC_CACHE = {}


def kernel(**inputs):
    stage = DEBUG_STAGE
    if stage not in _NC_CACHE:
        _NC_CACHE[stage] = build_program(stage)
    nc = _NC_CACHE[stage]
    maps = _prep_inputs(inputs)
    res = run_bass_kernel_spmd(nc, maps, core_ids=list(range(8)))
    outp = np.empty((4, 4096, D), np.float32)
    for core in range(8):
        b, s = core // 2, core % 2
        o = res.results[core]['out']
        if s == 0:
            outp[b, :NTOK] = o
        else:
            outp[b, NTOK:] = o[::-1]
    return outp
```

```python
import contextlib
import numpy as np
import concourse.bass as bass
import concourse.mybir as mybir
from concourse.bass_utils import run_bass_kernel_spmd

F32, BF16 = mybir.dt.float32, mybir.dt.bfloat16
AF = mybir.ActivationFunctionType
ALU = mybir.AluOpType
AX = mybir.AxisListType
ENG = ('pe', 'dve', 'act', 'pool', 'sp')

D = 1024; NTOK = 2048; NCH = 16; NEXT = 2304; NCHX = 18; DFF = 4096
AB_IN = 5664; ML_IN = 6176
NEG = -30000.0
DEBUG_STAGE = None


def mk(m, *a, **kw):
    return lambda e: getattr(e, m)(*a, **kw)


class Sched:
    def __init__(self, nc, es):
        self.nc, self.es = nc, es
        self.q = {e: [] for e in ENG}
        self.sems, self.cnt = {}, {}
        for e in ENG:
            self._mk('e_' + e)
        self.seen = {e: {} for e in ENG}
        self.lastw, self.readers = {}, {}

    def _mk(self, name):
        self.sems[name] = self.es.enter_context(self.nc.semaphore(name))
        self.cnt[name] = 0

    def _collect(self, eng, r, w):
        deps = {}

        def add(tok):
            if tok is None:
                return
            s, v = tok
            if s == 'e_pe' and eng == 'pe':
                return
            if deps.get(s, 0) < v:
                deps[s] = v
        for k in r:
            add(self.lastw.get(k))
        for k in w:
            add(self.lastw.get(k))
            for s, v in self.readers.get(k, {}).items():
                add((s, v))
        waits = []
        for s, v in deps.items():
            if self.seen[eng].get(s, 0) < v:
                waits.append((s, v))
                self.seen[eng][s] = v
        return waits

    def _commit(self, tok, r, w):
        for k in r:
            d = self.readers.setdefault(k, {})
            if d.get(tok[0], 0) < tok[1]:
                d[tok[0]] = tok[1]
        for k in w:
            self.lastw[k] = tok
            self.readers[k] = {}

    def op(self, eng, fn, r=(), w=()):
        waits = self._collect(eng, r, w)
        s = 'e_' + eng
        self.cnt[s] += 1
        self.q[eng].append((waits, fn, s, 1))
        self._commit((s, self.cnt[s]), r, w)

    def dma(self, eng, fn, slot, r=(), w=(), inc=16):
        if slot not in self.sems:
            self._mk(slot)
        waits = self._collect(eng, r, w)
        self.cnt[slot] += inc
        self.q[eng].append((waits, fn, slot, inc))
        self._commit((slot, self.cnt[slot]), r, w)

    def barrier(self):
        for e in ENG:
            waits = []
            for s, c in self.cnt.items():
                if c > 0 and self.seen[e].get(s, 0) < c and not (s == 'e_pe' and e == 'pe'):
                    waits.append((s, c))
                    self.seen[e][s] = c
            self.q[e].append((waits, None, None, 0))

    def replay(self, block):
        def run(name):
            def f(e):
                for waits, fn, s, inc in self.q[name]:
                    for ws, wv in waits:
                        e.wait_ge(self.sems[ws], wv)
                    if fn is not None:
                        ins = fn(e)
                        if inc == 1 and not s.startswith('e_'):
                            ins.then_inc(self.sems[s])
                        else:
                            ins.then_inc(self.sems[s], inc)
            return f
        block.tensor(run('pe'))
        block.vector(run('dve'))
        block.scalar(run('act'))
        block.gpsimd(run('pool'))
        block.sync(run('sp'))


class WStream:
    NSLOT = 2
    SLOT = 6144

    def __init__(self, S, tiles, order, I):
        self.S, self.tiles, self.order, self.I = S, tiles, order, I
        self.rec = []
        self.issued = 0
        self.i = 0

    def _issue(self, idx):
        spec = (self.order if self.order is not None else self.rec)[idx]
        slot = idx % self.NSLOT
        t = self.tiles[slot]
        off = 0
        for (nm, ix, kt, n) in spec:
            src = self.I[nm][ix]
            dst = t[:, off:off + kt * n].rearrange("p (k n) -> p k n", k=kt)
            srcv = src.rearrange("(k p) n -> p k n", p=128)
            self.S.dma('pool', (mk('dma_start', out=dst, in_=srcv)),
                       'w%d' % slot, w=[('w', slot)])
            off += kt * n

    def get(self, spec, free_prev=True):
        idx = self.i
        self.i += 1
        self.rec.append(spec)
        if self.order is None:
            self._issue(idx)
            self.issued = idx + 1
        else:
            lim = idx + self.NSLOT if free_prev else idx + 1
            while self.issued < min(len(self.order), lim):
                self._issue(self.issued)
                self.issued += 1
        slot = idx % self.NSLOT
        t = self.tiles[slot]
        outs, off = [], 0
        for (nm, ix, kt, n) in spec:
            outs.append(t[:, off:off + kt * n].rearrange("p (k n) -> p k n", k=kt))
            off += kt * n
        assert off <= self.SLOT, off
        return outs, ('w', slot)


def build_program(stage=None):
    nc = bass.Bass("TRN2", target_bir_lowering=False)
    din = lambda name, shape: nc.dram_tensor(name, list(shape), F32, kind="ExternalInput").ap()
    I = {}
    I['x'] = din('x', [NEXT, D])
    I['cvec'] = din('cvec', [128, 8])
    I['ada_w'] = din('ada_w', [2, D, 6 * D])
    I['ada_b'] = din('ada_b', [2, 128, 6 * D])
    I['ngb'] = din('ngb', [2, 4, 128, D])
    I['mlp_w1'] = din('mlp_w1', [2, D, DFF])
    I['mlp_w2'] = din('mlp_w2', [2, DFF, D])
    I['ml_w'] = din('ml_w', [D, ML_IN])
    I['ml_gb'] = din('ml_gb', [128, 32])
    I['ml_hn'] = din('ml_hn', [128, 2048])
    I['ml_wo'] = din('ml_wo', [2048, D])
    I['sel'] = din('sel', [128, 2])
    I['ml_wg'] = din('ml_wg', [D, 32])
    I['ab_w'] = din('ab_w', [D, AB_IN])
    I['ab_wo'] = din('ab_wo', [2048, D])
    I['ab_wdt'] = din('ab_wdt', [D, 32])
    I['ab_dtb'] = din('ab_dtb', [128, 32])
    I['ab_alog'] = din('ab_alog', [128, 32])
    I['ab_dsk'] = din('ab_dsk', [128, 16])
    I['ab_sn'] = din('ab_sn', [128, 8])
    I['ab_cw'] = din('ab_cw', [128, 12, 5])
    I['ab_cb'] = din('ab_cb', [128, 12])
    I['ab_bias'] = din('ab_bias', [16, 3, 128, 640])
    out = nc.dram_tensor('out', [NTOK, D], F32, kind="ExternalOutput").ap()
    xs = nc.dram_tensor('xs', [NTOK, D], F32).ap()
    modv = nc.dram_tensor('modv', [2, 6, 128, D], F32).ap()
    cc_in = nc.dram_tensor('cc_in', [128, 258], F32).ap()
    cc_out = nc.dram_tensor('cc_out', [256, 258], F32).ap()
    ss_in = nc.dram_tensor('ss_in', [128, 512], F32).ap()
    ss_out = nc.dram_tensor('ss_out', [256, 512], F32).ap()
    sa_dram = nc.dram_tensor('sa_dram', [NCH, 128, 512], BF16).ap()

    order = None
    for pass_i in range(2):
        if pass_i == 1:
            pass
    order = _emit(None, I, out, xs, modv, (cc_in, ss_in, sa_dram), (cc_out, ss_out), None, dry=True, stage=stage)
    _emit(nc, I, out, xs, modv, (cc_in, ss_in, sa_dram), (cc_out, ss_out), order, dry=False, stage=stage)
    return nc


class _Dry:
    def __getattr__(self, k):
        return self
    def __call__(self, *a, **k):
        return self
    def __getitem__(self, k):
        return self
    def __enter__(self):
        return self
    def __exit__(self, *a):
        return False


def _emit(nc, I, out, xs, modv, cc_in, cc_out, order, dry, stage):
    if dry:
        nc = _Dry()
        I = {k: _Dry() for k in I}
        out = xs = modv = _Dry()
        cc_in = cc_out = (_Dry(), _Dry(), _Dry())
    with contextlib.ExitStack() as es:
        S = Sched(nc, es)
        ml_ccin, ml_ccout = cc_in[0], cc_out[0]
        ss_ccin, ss_ccout = cc_in[1], cc_out[1]
        sa_d = cc_in[2]
        uid = [0]

        def sb(name, shape, dt=F32, st=es):
            uid[0] += 1
            return st.enter_context(nc.sbuf_tensor('%s_%d' % (name, uid[0]), list(shape), dt))
        hT = sb('hT', [128, 8, NEXT], BF16)
        acc = sb('acc', [128, NCH, D], F32)
        wt = [sb('wt%d' % i, [128, WStream.SLOT], BF16) for i in range(WStream.NSLOT)]
        W = WStream(S, wt, order, I)
        ident_b = sb('ident_b', [128, 128], BF16)
        I_cst = None
        PSB = [es.enter_context(nc.psum_tensor('psb%d' % i, [128, 512], F32)) for i in range(5)]
        PS2 = es.enter_context(nc.psum_tensor('ps2', [128, 1024], F32))
        PST = es.enter_context(nc.psum_tensor('pst', [128, 1024], BF16))
        bank_rr = [0]

        def bank():
            i = bank_rr[0] % 4
            bank_rr[0] += 1
            return PSB[i], ('ps', i)

        if not dry:
            I_cst = nc.dram_tensor('cst_in', [128, 6, 128], F32, kind="ExternalInput").ap()
        else:
            I_cst = _Dry()
        cstall = sb('cstall', [128, 6, 128], F32)
        S.dma('sp', mk('dma_start', out=cstall[:], in_=I_cst), 'd_cst', w=['cst'])
        S.op('dve', mk('tensor_copy', out=ident_b[:], in_=cstall[:, 0, :]), r=['cst'], w=['identb'])
        identf = cstall[:, 0, :]
        tri = cstall[:, 1, :]
        triT = cstall[:, 2, :]
        mneg = cstall[:, 3, :]
        mnegT = cstall[:, 4, :]
        ones_f = cstall[:, 5, :]

        epsc = sb('epsc', [128, 1], F32)
        S.op('dve', mk('memset', epsc[:], 1e-6), w=['epsc'])
        onec = sb('onec', [128, 1], F32)
        S.op('dve', mk('memset', onec[:], 1.0), w=['onec'])

        def rms_stats(src_ap, rkeys, ph, tag):
            junk = ph['junk']; ssq = ph['ssq']; rstd = ph['rstd']
            S.op('dve', mk('memset', ssq[:], 0.0), w=['ssq'])
            S.op('act', mk('activation', out=junk[:], in_=src_ap, func=AF.Square, accum_out=ssq[:]),
                 r=list(rkeys) + ['ssq'], w=['junk', 'ssq'])
            S.op('act', mk('activation', out=rstd[:], in_=ssq[:], func=AF.Sqrt, bias=epsc[:], scale=1.0 / D),
                 r=['ssq', 'epsc'], w=['rstd'])
            S.op('dve', mk('reciprocal', out=rstd[:], in_=rstd[:]), r=['rstd'], w=['rstd'])
            return rstd

        def prologue(c, src_ap, rkeys, ph, A, Sv):
            rstd = rms_stats(src_ap, rkeys, ph, 'p')
            tmp = ph['tmp']; hn = ph['hn']
            S.op('dve', mk('scalar_tensor_tensor', out=tmp[:], in0=src_ap, scalar=rstd[:, 0:1], in1=A[:],
                                                         op0=ALU.mult, op1=ALU.mult),
                 r=list(rkeys) + ['rstd', 'vecA'], w=['tmp'])
            S.op('dve', mk('tensor_tensor', out=hn[:], in0=tmp[:], in1=Sv[:], op=ALU.add),
                 r=['tmp', 'vecS'], w=['hn'])
            for kt in range(8):
                S.op('pe', mk('transpose', out=PST[:, kt * 128:(kt + 1) * 128],
                                                        in_=hn[:, kt * 128:(kt + 1) * 128], identity=ident_b[:]),
                     r=['hn', 'identb'], w=['pst'])
            S.op('act', mk('copy', out=hT[:, :, c * 128:(c + 1) * 128],
                                         in_=PST[:].rearrange("p (k n) -> p k n", k=8)),
                 r=['pst'], w=[('hT', c)])

        def load_vec(dst, l, j, key):
            S.dma('sp', mk('dma_start', out=dst[:], in_=modv[l, j]), 'd_' + key, r=[('modv', l, j)], w=[key])

        def epilogue(l, which, src_x, src_key, dst, ph, nxt):
            G = ph['G']
            load_vec(G, l, 2 if which == 0 else 5, 'vecG')
            if nxt is not None:
                nl, nj = nxt
                load_vec(ph['A'], nl, nj[0], 'vecA')
                load_vec(ph['S'], nl, nj[1], 'vecS')
            for c in range(NCH):
                xb = ph['xb'][c % 2]
                xk = ('xb', c % 2)
                S.dma('sp', mk('dma_start', out=xb[:], in_=src_x[c * 128:(c + 1) * 128, :]),
                      'xl%d' % (c % 2), r=[(src_key, c)], w=[xk])
                rstd = rms_stats(acc[:, c, :], [('acc', c)], ph, 'e')
                tmp = ph['tmp']
                S.op('dve', mk('scalar_tensor_tensor', out=tmp[:], in0=acc[:, c, :], scalar=rstd[:, 0:1],
                                                                  in1=G[:], op0=ALU.mult, op1=ALU.mult),
                     r=[('acc', c), 'rstd', 'vecG'], w=['tmp'])
                S.op('dve', mk('tensor_tensor', out=xb[:], in0=xb[:], in1=tmp[:], op=ALU.add),
                     r=['tmp', xk], w=[xk])
                dkey = ('xs', c) if dst is xs else ('out', c)
                S.dma('sp', mk('dma_start', out=dst[c * 128:(c + 1) * 128, :], in_=xb[:]),
                      'xst%d' % (c % 2), r=[xk], w=[dkey])
                if nxt is not None:
                    prologue(c, xb[:], [xk], ph, ph['A'], ph['S'])

        def acc_mm(c, lhs_list, wtiles, wkey, first):
            for half in range(2):
                ps, pk = bank()
                n = len(lhs_list)
                for i, (lap, lk) in enumerate(lhs_list):
                    S.op('pe', mk('matmul',
                        ps[:, :], lhsT=lap, rhs=wtiles[i][:, half * 512:(half + 1) * 512],
                        start=(i == 0), stop=(i == n - 1)), r=list(lk) + [wkey], w=[pk])
                dst = acc[:, c, half * 512:(half + 1) * 512]
                if first:
                    S.op('dve', mk('tensor_copy', out=dst, in_=ps[:, :]), r=[pk], w=[('acc', c)])
                else:
                    S.op('dve', mk('tensor_tensor', out=dst, in0=ps[:, :], in1=dst, op=ALU.add),
                         r=[pk, ('acc', c)], w=[('acc', c)])

        def alloc_norm_bufs(ph_es):
            ph = {}
            ph['junk'] = sb('junk', [128, D], BF16, ph_es)
            ph['ssq'] = sb('ssq', [128, 1], F32, ph_es)
            ph['rstd'] = sb('rstd', [128, 1], F32, ph_es)
            ph['tmp'] = sb('tmp', [128, D], F32, ph_es)
            ph['hn'] = sb('hn', [128, D], BF16, ph_es)
            ph['A'] = sb('vA', [128, D], F32, ph_es)
            ph['S'] = sb('vS', [128, D], F32, ph_es)
            ph['G'] = sb('vG', [128, D], F32, ph_es)
            ph['xb'] = [sb('xb%d' % i, [128, D], F32, ph_es) for i in range(2)]
            return ph

        with contextlib.ExitStack() as pe0:
            cf = sb('cf', [128, 8], F32, pe0)
            cb = sb('cb', [128, 8], BF16, pe0)
            crep = sb('crep', [128, 8, 128], BF16, pe0)
            adab = sb('adab', [128, D], F32, pe0)
            ngt = sb('ngt', [128, D], F32, pe0)
            mv = [sb('mv%d' % i, [128, D], F32, pe0) for i in range(2)]
            S.dma('sp', mk('dma_start', out=cf[:], in_=I['cvec']), 'd_cf', w=['cf'])
            S.op('act', mk('activation', out=cb[:], in_=cf[:], func=AF.Silu), r=['cf'], w=['cb'])
            S.op('dve', mk('tensor_copy', out=crep[:], in_=cb[:].unsqueeze(2).to_broadcast([128, 8, 128])),
                 r=['cb'], w=['crep'])
            n_mod = 0
            for l in range(2):
                for j in range(6):
                    S.dma('sp', mk('dma_start', out=adab[:], in_=I['ada_b'][l, :, j * D:(j + 1) * D]),
                          'd_adab', w=['adab'])
                    gi = {1: 0, 2: 1, 4: 2, 5: 3}.get(j)
                    if gi is not None:
                        S.dma('sp', mk('dma_start', out=ngt[:], in_=I['ngb'][l, gi]), 'd_ngt', w=['ngt'])
                    m = mv[n_mod % 2]
                    mvk = ('mv', n_mod % 2)
                    n_mod += 1
                    for half in range(2):
                        (wa,), wk = W.get([('ada_w', (l, slice(None), slice(j * D + half * 512, j * D + (half + 1) * 512)), 8, 512)])
                        ps, pk = bank()
                        for kt in range(8):
                            S.op('pe', mk('matmul', ps[:, :], lhsT=crep[:, kt, :], rhs=wa[:, kt, :],
                                start=(kt == 0), stop=(kt == 7)), r=['crep', wk], w=[pk])
                        hs = slice(half * 512, (half + 1) * 512)
                        S.op('dve', mk('tensor_tensor', out=m[:, hs], in0=ps[:, :], in1=adab[:, hs],
                                                                                op=ALU.add), r=[pk, 'adab'], w=[mvk])
                    if j in (1, 4):
                        S.op('dve', mk('scalar_tensor_tensor', out=m[:], in0=m[:], scalar=1.0, in1=ngt[:],
                                                                          op0=ALU.add, op1=ALU.mult),
                             r=[mvk, 'ngt'], w=[mvk])
                    elif j in (2, 5):
                        S.op('dve', mk('tensor_tensor', out=m[:], in0=m[:], in1=ngt[:], op=ALU.mult),
                             r=[mvk, 'ngt'], w=[mvk])
                    S.dma('sp', mk('dma_start', out=modv[l, j], in_=m[:]), 'mvst%d' % mvk[1],
                          r=[mvk], w=[('modv', l, j)])
        S.barrier()

        with contextlib.ExitStack() as p1:
            ph = alloc_norm_bufs(p1)
            load_vec(ph['A'], 0, 1, 'vecA')
            load_vec(ph['S'], 0, 0, 'vecS')
            for c in range(NCHX):
                xb = ph['xb'][c % 2]
                xk = ('xb', c % 2)
                S.dma('sp', mk('dma_start', out=xb[:], in_=I['x'][c * 128:(c + 1) * 128, :]),
                      'xl%d' % (c % 2), w=[xk])
                prologue(c, xb[:], [xk], ph, ph['A'], ph['S'])
        S.barrier()

        def zero_acc():
            for c in range(NCH):
                S.op('dve', mk('memset', acc[:, c, :], 0.0), w=[('acc', c)])

        def mlp(l):
            with contextlib.ExitStack() as pm:
                uT = [sb('uT%d' % i, [128, 4, NTOK], BF16, pm) for i in range(2)]
                rl = [sb('rl%d' % i, [128, 512], F32, pm) for i in range(2)]
                hkeys = [('hT', c) for c in range(NCH)]
                for blk in range(8):
                    u = uT[blk % 2]
                    uk = ('uT', blk % 2)
                    (w1,), w1k = W.get([('mlp_w1', (l, slice(None), slice(blk * 512, (blk + 1) * 512)), 8, 512)])
                    nrl = 0
                    for ft in range(4):
                        for tb in range(4):
                            ps, pk = bank()
                            for kt in range(8):
                                S.op('pe', mk('matmul',
                                    ps[:, :], lhsT=w1[:, kt, ft * 128:(ft + 1) * 128],
                                    rhs=hT[:, kt, tb * 512:(tb + 1) * 512], start=(kt == 0), stop=(kt == 7)),
                                    r=hkeys[tb * 4:tb * 4 + 4] + [w1k], w=[pk])
                            r_ = rl[nrl % 2]
                            rk = ('rl', nrl % 2)
                            nrl += 1
                            S.op('act', mk('activation', out=r_[:], in_=ps[:, :], func=AF.Relu),
                                 r=[pk], w=[rk])
                            S.op('dve', mk('tensor_tensor',
                                out=u[:, ft, tb * 512:(tb + 1) * 512], in0=r_[:], in1=r_[:], op=ALU.mult),
                                r=[rk], w=[uk])
                    (w2,), w2k = W.get([('mlp_w2', (l, slice(blk * 512, (blk + 1) * 512), slice(None)), 4, D)])
                    for c in range(NCH):
                        acc_mm(c, [(u[:, ft, c * 128:(c + 1) * 128], [uk]) for ft in range(4)],
                               [w2[:, ft, :] for ft in range(4)], w2k, first=(blk == 0))
            S.barrier()

        def run_epilogue(l, which, src_x, src_key, dst, nxt):
            with contextlib.ExitStack() as pp:
                ph = alloc_norm_bufs(pp)
                epilogue(l, which, src_x, src_key, dst, ph, nxt)
            S.barrier()


        def mlstm():
            with contextlib.ExitStack() as pm:
                hk = [('hT', c) for c in range(NCH)]
                qT = sb('qT', [128, NTOK], BF16, pm)
                kT = sb('kT', [128, NTOK], BF16, pm)
                ktm = sb('ktm', [128, NCH, 128], BF16, pm)
                v1 = sb('v1', [128, NCH, 260], BF16, pm)
                CbA = sb('CbA', [128, NCH, 260], BF16, pm)
                CbB = sb('CbB', [128, NCH, 260], BF16, pm)
                mA = sb('mA', [128, NCH], F32, pm)
                mB = sb('mB', [128, NCH], F32, pm)
                Cn = sb('Cn', [128, 260], F32, pm)
                mcur = sb('mcur', [128, 1], F32, pm)
                gates = sb('gates', [128, NCH, 32], F32, pm)
                nl = sb('nl', [128, NCH, 16], F32, pm)
                nb = sb('nb', [128, NCH, 16], F32, pm)
                u = sb('u', [128, NCH, 16], F32, pm)
                ngr = sb('ngr', [128, NCH, 16], F32, pm)
                umx = sb('umx', [128, NCH, 16], F32, pm)
                cmx = sb('cmx', [128, NCH, 16], F32, pm)
                gb = sb('gb', [128, 32], F32, pm)
                hnw = sb('hnw', [128, 256], F32, pm)
                sel = sb('sel', [128, 2], F32, pm)
                wg = sb('wg', [128, 8, 32], BF16, pm)
                sm = sb('sm', [128, 16], F32, pm)
                hsum = sb('hsum', [128, 256], F32, pm)
                so = sb('so', [128, 256], F32, pm)
                yb = sb('yb', [128, 256], BF16, pm)
                yT = sb('yT', [128, 2, 128], BF16, pm)
                xch = sb('xch', [128, 2, 258], F32, pm)
                stx = sb('stx', [128, 258], F32, pm)
                P4, P5, P6 = PSB[4], PS2[:, 0:512], PS2[:, 512:1024]

                S.dma('sp', mk('dma_start', out=gb[:], in_=I['ml_gb']), 'd_gb', w=['gb'])
                S.dma('sp', mk('dma_start', out=sel[:], in_=I['sel']), 'd_sel', w=['sel'])
                S.dma('pool', mk('dma_start', out=wg[:], in_=I['ml_wg'].rearrange("(k p) n -> p k n", p=128)),
                      'wgl', w=['wg'])
                S.op('dve', mk('memset', v1[:, :, 256:260], 1.0), w=['v1'])
                for c in range(NCH):
                    for kt in range(8):
                        S.op('pe', mk('matmul', P4[:, 0:32], lhsT=hT[:, kt, c * 128:(c + 1) * 128],
                                                                  rhs=wg[:, kt, :], start=(kt == 0), stop=(kt == 7)),
                             r=[hk[c], 'wg'], w=['p4b'])
                    S.op('dve', mk('tensor_tensor', out=gates[:, c, :], in0=P4[:, 0:32], in1=gb[:], op=ALU.add),
                         r=['p4b', 'gb'], w=['gates'])
                for d in range(2):
                    S.op('act', mk('activation', out=nl[:, :, d * 8:(d + 1) * 8], in_=gates[:, :, 16 * d + 8:16 * d + 16],
                                                            func=AF.Exp, scale=-1.0), r=['gates'], w=['nl'])
                S.op('act', mk('activation', out=nl[:], in_=nl[:], func=AF.Ln, bias=onec[:], scale=1.0),
                     r=['nl', 'onec'], w=['nl'])
                for c in range(NCH):
                    S.op('pe', mk('matmul', P4[:, 0:8], lhsT=tri, rhs=nl[:, c, 0:8], start=True, stop=True),
                         r=['nl', 'cst'], w=['p4b'])
                    S.op('pe', mk('matmul', P4[:, 8:16], lhsT=triT, rhs=nl[:, c, 8:16], start=True, stop=True),
                         r=['nl', 'cst'], w=['p4b'])
                    S.op('pe', mk('matmul', P4[:, 16:32], lhsT=ones_f, rhs=nl[:, c, :], start=True, stop=True),
                         r=['nl', 'cst'], w=['p4b'])
                    S.op('dve', mk('tensor_copy', out=nb[:, c, :], in_=P4[:, 0:16]), r=['p4b'], w=['nb'])
                    S.op('dve', mk('tensor_copy', out=ngr[:, c, :], in_=P4[:, 16:32]), r=['p4b'], w=['ngr'])
                    for d in range(2):
                        S.op('dve', mk('tensor_tensor', out=u[:, c, d * 8:(d + 1) * 8], in0=nb[:, c, d * 8:(d + 1) * 8],
                                                                        in1=gates[:, c, 16 * d:16 * d + 8], op=ALU.add),
                             r=['nb', 'gates'], w=['u'])
                dg4 = [sb('dg4s', [128, 4, 128], F32, pm)] * 2
                Ep4 = [sb('Ep4s', [128, 4, 128], F32, pm)] * 2
                nq = 0
                for c in range(NCH):
                    for jq in range(4):
                        j0 = jq * 4
                        d = j0 // 8
                        r_ = 0
                        nq += 1
                        ps, pk = bank()
                        for i in range(4):
                            S.op('dve', mk('tensor_scalar_mul', out=dg4[r_][:, i, :], in0=identf, scalar1=u[:, c, j0 + i:j0 + i + 1]),
                                 r=['u', 'cst'], w=[('dg4', r_, i)])
                            S.op('pe', mk('matmul', ps[:, i * 128:(i + 1) * 128], lhsT=ones_f, rhs=dg4[r_][:, i, :], start=True, stop=True),
                                 r=[('dg4', r_, i), 'cst'], w=[pk])
                        for i in range(4):
                            S.op('dve', mk('reduce_max', out=umx[:, c, j0 + i:j0 + i + 1], in_=ps[:, i * 128:(i + 1) * 128], axis=AX.X),
                                 r=[pk], w=['umx'])
                        msk = mnegT if d == 0 else mneg
                        S.op('dve', mk('tensor_tensor', out=Ep4[r_][:], in0=ps[:, :].rearrange("p (i t) -> p i t", i=4),
                                       in1=msk.unsqueeze(1).to_broadcast([128, 4, 128]), op=ALU.add),
                             r=[pk, 'cst'], w=[('Ep4', r_)])
                        for i in range(4):
                            S.op('dve', mk('reduce_max', out=cmx[:, c, j0 + i:j0 + i + 1], in_=Ep4[r_][:, i, :], axis=AX.X),
                                 r=[('Ep4', r_)], w=['cmx'])

                smS = [sb('smS%d' % i_, [128, 8], F32, pm) for i_ in range(2)]
                kwr = [sb('kwr%d' % i_, [128, 128], BF16, pm) for i_ in range(2)]
                t5s = [sb('t5ss', [128, 260], F32, pm)] * 2

                def state_prep(c, j):
                    r_ = c % 2
                    sm_, kw_ = smS[r_], kwr[r_]
                    S.op('dve', mk('tensor_scalar_mul', out=sm_[:, 0:1], in0=umx[:, c, j:j + 1], scalar1=-1.0), r=['umx'], w=[('smS', r_)])
                    S.op('act', mk('activation', out=sm_[:, 6:7], in_=u[:, c, j:j + 1], func=AF.Exp, bias=sm_[:, 0:1], scale=1.0),
                         r=['u', ('smS', r_)], w=[('smS', r_)])
                    S.op('dve', mk('tensor_scalar_mul', out=kw_[:], in0=ktm[:, c, :], scalar1=sm_[:, 6:7]), r=['ktm', ('smS', r_)], w=[('kwr', r_)])
                    ps, pk = bank()
                    S.op('pe', mk('matmul', ps[:, 0:257], lhsT=kw_[:], rhs=v1[:, c, 0:257], start=True, stop=True),
                         r=[('kwr', r_), 'v1'], w=[pk])
                    return ps, pk

                def state_chain(c, j, Cb, mS, ps, pk):
                    r_ = c % 2
                    sm_, t5_ = smS[r_], t5s[r_]
                    S.op('act', mk('copy', out=Cb[:, c, 0:257], in_=Cn[:, 0:257]), r=['Cn'], w=['Cb'])
                    S.op('dve', mk('tensor_copy', out=mS[:, c:c + 1], in_=mcur[:]), r=['mcur'], w=['mS'])
                    S.op('dve', mk('tensor_tensor', out=sm_[:, 1:2], in0=mcur[:], in1=umx[:, c, j:j + 1], op=ALU.max),
                         r=['mcur', 'umx', ('smS', r_)], w=[('smS', r_)])
                    S.op('dve', mk('tensor_tensor', out=sm_[:, 2:3], in0=mcur[:], in1=sm_[:, 1:2], op=ALU.subtract),
                         r=['mcur', ('smS', r_)], w=[('smS', r_)])
                    S.op('dve', mk('tensor_tensor', out=sm_[:, 3:4], in0=umx[:, c, j:j + 1], in1=sm_[:, 1:2], op=ALU.subtract),
                         r=['umx', ('smS', r_)], w=[('smS', r_)])
                    S.op('act', mk('activation', out=sm_[:, 4:6], in_=sm_[:, 2:4], func=AF.Exp), r=[('smS', r_)], w=[('smS', r_)])
                    S.op('act', mk('mul', out=t5_[:, 0:257], in_=ps[:, 0:257], mul=sm_[:, 5:6]), r=[pk, ('smS', r_)], w=[('t5s', 0)])
                    S.op('dve', mk('scalar_tensor_tensor', out=Cn[:, 0:257], in0=Cn[:, 0:257], scalar=sm_[:, 4:5], in1=t5_[:, 0:257],
                                   op0=ALU.mult, op1=ALU.add), r=['Cn', ('smS', r_), ('t5s', 0)], w=['Cn'])
                    S.op('dve', mk('tensor_tensor', out=mcur[:], in0=sm_[:, 1:2], in1=ngr[:, c, j:j + 1], op=ALU.subtract),
                         r=[('smS', r_), 'ngr'], w=['mcur'])

                def state_pass(chunks, j, Cb, mS):
                    pend = [state_prep(chunks[0], j)]
                    for i_, c in enumerate(chunks):
                        if i_ + 1 < len(chunks):
                            pend.append(state_prep(chunks[i_ + 1], j))
                        ps, pk = pend.pop(0)
                        state_chain(c, j, Cb, mS, ps, pk)

                smO = [sb('smO%d' % i_, [128, 8], F32, pm) for i_ in range(2)]
                dgo = [sb('dgo%d' % i_, [128, 128], F32, pm) for i_ in range(2)]
                t2r = [sb('t2r%d' % i_, [128, 128], F32, pm) for i_ in range(2)]
                scTr = [sb('scTr%d' % i_, [128, 128], BF16, pm) for i_ in range(2)]
                t5o = [sb('t5o%d' % i_, [128, 260], F32, pm) for i_ in range(2)]
                ndr = [sb('ndr%d' % i_, [128, 260], F32, pm) for i_ in range(2)]

                def out_dir_steps(c, j, Cb, mS):
                    d = j // 8
                    sm_, dg_, t2_, sc_, t5_, nd_ = smO[d], dgo[d], t2r[d], scTr[d], t5o[d], ndr[d]
                    K = lambda n: (n, d)
                    R2 = P5[:, 0:128] if d == 0 else P6[:, 0:128]
                    r2k = ('ps2b', d)
                    msk = mneg if d == 0 else mnegT
                    hold = {}
                    st = []
                    st.append(lambda: S.op('dve', mk('tensor_tensor', out=sm_[:, 0:1], in0=cmx[:, c, j:j + 1], in1=mS[:, c:c + 1], op=ALU.max),
                                           r=['cmx', 'mS', K('smO')], w=[K('smO')]))
                    st.append(lambda: S.op('dve', mk('tensor_scalar_mul', out=dg_[:], in0=identf, scalar1=sm_[:, 0:1]),
                                           r=[K('smO'), 'cst'], w=[K('dgo')]))
                    st.append(lambda: S.op('pe', mk('matmul', R2, lhsT=ones_f, rhs=dg_[:], start=True, stop=True),
                                           r=[K('dgo'), 'cst'], w=[r2k]))
                    st.append(lambda: S.op('dve', mk('tensor_tensor', out=sm_[:, 2:3], in0=mS[:, c:c + 1], in1=sm_[:, 0:1], op=ALU.subtract),
                                           r=['mS', K('smO')], w=[K('smO')]))
                    st.append(lambda: S.op('dve', mk('tensor_tensor', out=sm_[:, 4:5], in0=nb[:, c, j:j + 1], in1=sm_[:, 0:1], op=ALU.subtract),
                                           r=['nb', K('smO')], w=[K('smO')]))
                    st.append(lambda: S.op('dve', mk('tensor_tensor', out=t2_[:], in0=msk, in1=R2, op=ALU.subtract),
                                           r=[r2k, 'cst'], w=[K('t2r')]))
                    st.append(lambda: S.op('act', mk('activation', out=t2_[:], in_=t2_[:], func=AF.Exp, bias=u[:, c, j:j + 1], scale=1.0),
                                           r=[K('t2r'), 'u'], w=[K('t2r')]))
                    st.append(lambda: S.op('act', mk('activation', out=sm_[:, 3:4], in_=sm_[:, 2:3], func=AF.Exp), r=[K('smO')], w=[K('smO')]))
                    st.append(lambda: S.op('act', mk('activation', out=sm_[:, 5:6], in_=sm_[:, 4:5], func=AF.Exp), r=[K('smO')], w=[K('smO')]))
                    st.append(lambda: S.op('dve', mk('tensor_tensor', out=sc_[:], in0=P4[:, 256:384], in1=t2_[:], op=ALU.mult),
                                           r=['p4b', K('t2r')], w=[K('scTr')]))

                    def mm_nd():
                        ps4, pk4 = bank()
                        ps5, pk5 = bank()
                        hold['p'] = (ps4, pk4, ps5, pk5)
                        S.op('pe', mk('matmul', ps4[:, 0:257], lhsT=sc_[:], rhs=v1[:, c, 0:257], start=True, stop=True),
                             r=[K('scTr'), 'v1'], w=[pk4])
                        S.op('pe', mk('matmul', ps5[:, 0:257], lhsT=qT[:, c * 128:(c + 1) * 128], rhs=Cb[:, c, 0:257],
                                      start=True, stop=True), r=['qT', 'Cb'], w=[pk5])
                    st.append(mm_nd)
                    st.append(lambda: S.op('act', mk('mul', out=t5_[:, 0:257], in_=hold['p'][2][:, 0:257], mul=sm_[:, 3:4]),
                                           r=[hold['p'][3], K('smO')], w=[K('t5o')]))
                    st.append(lambda: S.op('dve', mk('tensor_tensor', out=nd_[:, 0:257], in0=hold['p'][0][:, 0:257], in1=t5_[:, 0:257], op=ALU.add),
                                           r=[hold['p'][1], K('t5o')], w=[K('ndr')]))
                    st.append(lambda: S.op('act', mk('activation', out=sm_[:, 6:7], in_=nd_[:, 256:257], func=AF.Abs),
                                           r=[K('ndr'), K('smO')], w=[K('smO')]))
                    st.append(lambda: S.op('dve', mk('tensor_tensor', out=sm_[:, 6:7], in0=sm_[:, 6:7], in1=sm_[:, 5:6], op=ALU.max),
                                           r=[K('smO')], w=[K('smO')]))
                    st.append(lambda: S.op('dve', mk('reciprocal', out=sm_[:, 6:7], in_=sm_[:, 6:7]), r=[K('smO')], w=[K('smO')]))
                    return st

                def out_both(c, h):
                    sa_ = out_dir_steps(c, h, CbA, mA)
                    sb_ = out_dir_steps(c, 8 + h, CbB, mB)
                    for x_, y_ in zip(sa_, sb_):
                        x_()
                        y_()
                    S.op('dve', mk('tensor_scalar_mul', out=hsum[:], in0=ndr[0][:, 0:256], scalar1=smO[0][:, 6:7]),
                         r=[('ndr', 0), ('smO', 0)], w=['hsum'])
                    S.op('dve', mk('scalar_tensor_tensor', out=hsum[:], in0=ndr[1][:, 0:256], scalar=smO[1][:, 6:7], in1=hsum[:],
                                   op0=ALU.mult, op1=ALU.add), r=[('ndr', 1), ('smO', 1), 'hsum'], w=['hsum'])

                for h in range(8):
                    (wq, wk_, wv, wo_in), wkey = W.get([
                        ('ml_w', (slice(None), slice(h * 128, (h + 1) * 128)), 8, 128),
                        ('ml_w', (slice(None), slice(1024 + h * 128, 1024 + (h + 1) * 128)), 8, 128),
                        ('ml_w', (slice(None), slice(2048 + h * 256, 2048 + (h + 1) * 256)), 8, 256),
                        ('ml_w', (slice(None), slice(4096 + h * 256, 4096 + (h + 1) * 256)), 8, 256)])
                    (wout,), wokey2 = W.get([('ml_wo', (slice(h * 256, (h + 1) * 256), slice(None)), 2, D)], free_prev=False)
                    S.dma('sp', mk('dma_start', out=hnw[:], in_=I['ml_hn'][:, h * 256:(h + 1) * 256]), 'd_hnw', w=['hnw'])
                    for (wsrc, dstT, scl) in ((wq, qT, 128.0 ** -0.5), (wk_, kT, 1.0)):
                        for tb in range(4):
                            ps, pk = bank()
                            for kt in range(8):
                                S.op('pe', mk('matmul',
                                    ps[:, :], lhsT=wsrc[:, kt, :], rhs=hT[:, kt, tb * 512:(tb + 1) * 512],
                                    start=(kt == 0), stop=(kt == 7)), r=hk[tb * 4:tb * 4 + 4] + [wkey], w=[pk])
                            dk_ = 'qT' if scl != 1.0 else 'kT'
                            S.op('act', mk('mul', out=dstT[:, tb * 512:(tb + 1) * 512], in_=ps[:, :], mul=scl), r=[pk], w=[dk_])
                    for g8 in range(2):
                        for cc in range(8):
                            c = g8 * 8 + cc
                            S.op('pe', mk('transpose', out=PST[:, cc * 128:(cc + 1) * 128],
                                                                         in_=kT[:, c * 128:(c + 1) * 128], identity=ident_b[:]),
                                 r=['kT', 'identb'], w=['pst'])
                        S.op('act', mk('copy', out=ktm[:, g8 * 8:(g8 + 1) * 8, :],
                                                            in_=PST[:].rearrange("p (k n) -> p k n", k=8)), r=['pst'], w=['ktm'])
                    for c in range(NCH):
                        ps, pk = bank()
                        for kt in range(8):
                            S.op('pe', mk('matmul', ps[:, 0:256], lhsT=hT[:, kt, c * 128:(c + 1) * 128],
                                                                             rhs=wv[:, kt, :], start=(kt == 0), stop=(kt == 7)),
                                 r=[hk[c], wkey], w=[pk])
                        S.op('act', mk('copy', out=v1[:, c, 0:256], in_=ps[:, 0:256]), r=[pk], w=['v1'])
                    S.op('dve', mk('memset', Cn[:], 0.0), w=['Cn'])
                    S.op('dve', mk('memset', mcur[:], 0.0), w=['mcur'])
                    state_pass(list(range(NCH)), h, CbA, mA)
                    S.op('dve', mk('tensor_copy', out=stx[:, 0:257], in_=Cn[:, 0:257]), r=['Cn'], w=['stx'])
                    S.op('dve', mk('tensor_copy', out=stx[:, 257:258], in_=mcur[:]), r=['mcur', 'stx'], w=['stx'])
                    S.dma('sp', mk('dma_start', out=ml_ccin, in_=stx[:]), 'ccs', r=['stx'], w=['ccin'])
                    S.dma('pool', mk('collective_compute', "AllGather", ALU.bypass, replica_groups=[[0, 1], [2, 3], [4, 5], [6, 7]],
                                                                 ins=[ml_ccin], outs=[ml_ccout]), 'cc', r=['ccin'], w=['ccout'], inc=1)
                    S.dma('sp', mk('dma_start', out=xch[:], in_=ml_ccout.rearrange("(a p) n -> p a n", p=128)), 'ccl',
                          r=['ccout'], w=['xch'])
                    S.op('dve', mk('tensor_scalar_mul', out=stx[:], in0=xch[:, 0, :], scalar1=sel[:, 0:1]),
                         r=['xch', 'sel'], w=['stx'])
                    S.op('dve', mk('scalar_tensor_tensor', out=stx[:], in0=xch[:, 1, :], scalar=sel[:, 1:2], in1=stx[:],
                                                                 op0=ALU.mult, op1=ALU.add), r=['xch', 'sel', 'stx'], w=['stx'])
                    S.op('dve', mk('tensor_copy', out=Cn[:, 0:257], in_=stx[:, 0:257]), r=['stx'], w=['Cn'])
                    S.op('dve', mk('tensor_copy', out=mcur[:], in_=stx[:, 257:258]), r=['stx'], w=['mcur'])
                    state_pass(list(range(NCH - 1, -1, -1)), 8 + h, CbB, mB)
                    for c in range(NCH):
                        S.op('pe', mk('matmul', P4[:, 256:384], lhsT=kT[:, c * 128:(c + 1) * 128],
                                                           rhs=qT[:, c * 128:(c + 1) * 128], start=True, stop=True),
                             r=['kT', 'qT'], w=['p4b'])
                        out_both(c, h)
                        ps, pk = bank()
                        for kt in range(8):
                            S.op('pe', mk('matmul', ps[:, 0:256], lhsT=hT[:, kt, c * 128:(c + 1) * 128],
                                                                             rhs=wo_in[:, kt, :], start=(kt == 0), stop=(kt == 7)),
                                 r=[hk[c], wkey], w=[pk])
                        S.op('act', mk('activation', out=so[:], in_=ps[:, 0:256], func=AF.Sigmoid), r=[pk], w=['so'])
                        S.op('dve', mk('memset', sm[:, 15:16], 0.0), r=['sm'], w=['sm'])
                        S.op('act', mk('activation', out=yb[:], in_=hsum[:], func=AF.Square, accum_out=sm[:, 15:16]),
                             r=['hsum', 'sm'], w=['yb', 'sm'])
                        S.op('act', mk('activation', out=sm[:, 15:16], in_=sm[:, 15:16], func=AF.Sqrt, bias=epsc[:], scale=1.0 / 256),
                             r=['sm', 'epsc'], w=['sm'])
                        S.op('dve', mk('reciprocal', out=sm[:, 15:16], in_=sm[:, 15:16]), r=['sm'], w=['sm'])
                        S.op('dve', mk('scalar_tensor_tensor', out=hsum[:], in0=hsum[:], scalar=sm[:, 15:16],
                                                                          in1=hnw[:], op0=ALU.mult, op1=ALU.mult),
                             r=['hsum', 'sm', 'hnw'], w=['hsum'])
                        S.op('dve', mk('tensor_tensor', out=yb[:], in0=hsum[:], in1=so[:], op=ALU.mult),
                             r=['hsum', 'so'], w=['yb'])
                        for t in range(2):
                            S.op('pe', mk('transpose', out=PST[:, t * 128:(t + 1) * 128], in_=yb[:, t * 128:(t + 1) * 128],
                                                                  identity=ident_b[:]), r=['yb', 'identb'], w=['pst'])
                        S.op('act', mk('copy', out=yT[:], in_=PST[:, 0:256].rearrange("p (k n) -> p k n", k=2)),
                             r=['pst'], w=['yT'])
                        acc_mm(c, [(yT[:, 0, :], ['yT']), (yT[:, 1, :], ['yT'])], [wout[:, 0, :], wout[:, 1, :]], wokey2, first=(h == 0))
            S.barrier()


        def ssd():
            hk = [('hT', c) for c in range(NCHX)]
            P4 = PSB[4]
            with contextlib.ExitStack() as pm:
                dtb = sb('dtb', [128, 32], F32, pm)
                alog = sb('alog', [128, 32], F32, pm)
                dsk = sb('dsk', [128, 16], F32, pm)
                snc = sb('snc', [128, 8], F32, pm)
                cw = sb('cw', [128, 12, 5], F32, pm)
                cbv = sb('cbv', [128, 12], F32, pm)
                sel = sb('sel0', [128, 2], F32, pm)
                wdt = sb('wdt', [128, 8, 32], BF16, pm)
                dt = sb('dt', [128, NCH, 32], F32, pm)
                cs = sb('cs', [128, NCH, 32], F32, pm)
                ncs = sb('ncs', [128, NCH, 32], F32, pm)
                etot = sb('etot', [128, NCH, 32], F32, pm)
                w2 = sb('w2', [128, NCH, 32], F32, pm)
                ssq2 = sb('ssq2', [128, 2, NCH], F32, pm)
                for (t_, nm) in ((dtb, 'ab_dtb'), (alog, 'ab_alog'), (dsk, 'ab_dsk'), (snc, 'ab_sn'), (cbv, 'ab_cb'), (sel, 'sel')):
                    S.dma('sp', mk('dma_start', out=t_[:], in_=I[nm]), 'd_' + nm, w=[nm])
                S.dma('sp', mk('dma_start', out=cw[:], in_=I['ab_cw']), 'd_cw', w=['cw'])
                S.dma('pool', mk('dma_start', out=wdt[:], in_=I['ab_wdt'].rearrange("(k p) n -> p k n", p=128)), 'wgl', w=['wdt'])
                S.op('dve', mk('memset', ssq2[:], 0.0), w=['ssq2'])
                S.op('act', mk('activation', out=alog[:], in_=alog[:], func=AF.Exp), r=['ab_alog'], w=['ab_alog'])
                S.op('dve', mk('tensor_scalar_mul', out=alog[:], in0=alog[:], scalar1=-1.0), r=['ab_alog'], w=['ab_alog'])
                for c in range(NCH):
                    for kt in range(8):
                        S.op('pe', mk('matmul', P4[:, 0:32], lhsT=hT[:, kt, c * 128:(c + 1) * 128], rhs=wdt[:, kt, :],
                                      start=(kt == 0), stop=(kt == 7)), r=[hk[c], 'wdt'], w=[('p4', 0)])
                    S.op('dve', mk('tensor_tensor', out=dt[:, c, :], in0=P4[:, 0:32], in1=dtb[:], op=ALU.add),
                         r=[('p4', 0), 'ab_dtb'], w=['dt'])
                S.op('act', mk('activation', out=dt[:], in_=dt[:], func=AF.Exp), r=['dt'], w=['dt'])
                S.op('act', mk('activation', out=dt[:], in_=dt[:], func=AF.Ln, bias=onec[:], scale=1.0), r=['dt', 'onec'], w=['dt'])
                adt, tot = etot, w2
                S.op('dve', mk('tensor_tensor', out=adt[:], in0=dt[:], in1=alog[:].unsqueeze(1).to_broadcast([128, NCH, 32]), op=ALU.mult),
                     r=['dt', 'ab_alog'], w=['etot'])
                for c in range(NCH):
                    S.op('pe', mk('matmul', P4[:, 0:16], lhsT=tri, rhs=adt[:, c, 0:16], start=True, stop=True), r=['etot', 'cst'], w=[('p4', 0)])
                    S.op('pe', mk('matmul', P4[:, 16:32], lhsT=triT, rhs=adt[:, c, 16:32], start=True, stop=True), r=['etot', 'cst'], w=[('p4', 0)])
                    S.op('pe', mk('matmul', P4[:, 32:64], lhsT=ones_f, rhs=adt[:, c, :], start=True, stop=True), r=['etot', 'cst'], w=[('p4', 0)])
                    S.op('dve', mk('tensor_copy', out=cs[:, c, :], in_=P4[:, 0:32]), r=[('p4', 0)], w=['cs'])
                    S.op('dve', mk('tensor_copy', out=tot[:, c, :], in_=P4[:, 32:64]), r=[('p4', 0)], w=['w2'])
                S.op('dve', mk('tensor_scalar_mul', out=ncs[:], in0=cs[:], scalar1=-1.0), r=['cs'], w=['ncs'])
                S.op('act', mk('activation', out=etot[:], in_=tot[:], func=AF.Exp), r=['w2', 'etot'], w=['etot'])
                S.op('dve', mk('tensor_tensor', out=w2[:], in0=tot[:], in1=cs[:], op=ALU.subtract), r=['w2', 'cs'], w=['w2'])
                S.op('act', mk('activation', out=w2[:], in_=w2[:], func=AF.Exp), r=['w2'], w=['w2'])
                S.op('dve', mk('tensor_tensor', out=w2[:], in0=w2[:], in1=dt[:], op=ALU.mult), r=['w2', 'dt'], w=['w2'])

                xtm = sb('xtm', [128, NCH, 512], BF16, pm)
                Btm = sb('Btm', [128, NCH, 128], BF16, pm)
                BT = sb('BT', [128, NTOK], BF16, pm)
                CT = sb('CT', [128, NTOK], BF16, pm)
                Sst = sb('Sst', [128, 512], F32, pm)
                Sbf = sb('Sbf', [128, 512], BF16, pm)
                Xs = sb('Xs', [128, 512], BF16, pm)

                for g in range(2):
                    (wx, wB, wC), wkey = W.get([
                        ('ab_w', (slice(None), slice(1024 + g * 512, 1024 + (g + 1) * 512)), 8, 512),
                        ('ab_w', (slice(None), slice(2048 + g * 128, 2048 + (g + 1) * 128)), 8, 128),
                        ('ab_w', (slice(None), slice(2304 + g * 128, 2304 + (g + 1) * 128)), 8, 128)])
                    with contextlib.ExitStack() as pa:
                        cin = sb('cin', [128, 1028], F32, pa)
                        co = sb('co', [128, 1024], F32, pa)
                        xTt = sb('xTt', [128, NTOK], BF16, pa)
                        for ti in range(6):
                            wsrc = wx[:, :, ti * 128:(ti + 1) * 128] if ti < 4 else (wB if ti == 4 else wC)
                            ctile = (g * 4 + ti) if ti < 4 else (8 + g if ti == 4 else 10 + g)
                            dstT = xTt if ti < 4 else (BT if ti == 4 else CT)
                            dkey = 'xTt' if ti < 4 else ('BT' if ti == 4 else 'CT')
                            for hf in range(2):
                                base = hf * 1024 - 2
                                if hf == 0:
                                    S.op('dve', mk('memset', cin[:, 0:2], 0.0), r=['cin'], w=['cin'])
                                    pieces = [(0, 512), (512, 1024), (1024, 1026)]
                                else:
                                    pieces = [(1022, 1534), (1534, 2046), (2046, 2050)]
                                for (n0, n1) in pieces:
                                    ps, pk = bank()
                                    for kt in range(8):
                                        S.op('pe', mk('matmul', ps[:, 0:n1 - n0], lhsT=wsrc[:, kt, :], rhs=hT[:, kt, n0:n1],
                                                      start=(kt == 0), stop=(kt == 7)), r=hk[n0 // 128:(n1 - 1) // 128 + 1] + [wkey], w=[pk])
                                    S.op('act', mk('copy', out=cin[:, n0 - base:n1 - base], in_=ps[:, 0:n1 - n0]), r=[pk], w=['cin'])
                                S.op('dve', mk('tensor_scalar_mul', out=co[:], in0=cin[:, 0:1024], scalar1=cw[:, ctile, 0:1]),
                                     r=['cin', 'cw'], w=['co'])
                                for k in range(1, 5):
                                    S.op('dve', mk('scalar_tensor_tensor', out=co[:], in0=cin[:, k:k + 1024], scalar=cw[:, ctile, k:k + 1], in1=co[:],
                                                   op0=ALU.mult, op1=ALU.add), r=['cin', 'cw', 'co'], w=['co'])
                                S.op('act', mk('activation', out=dstT[:, hf * 1024:(hf + 1) * 1024], in_=co[:], func=AF.Silu,
                                               bias=cbv[:, ctile:ctile + 1], scale=1.0), r=['co', 'ab_cb'], w=[dkey])
                            if ti <= 4:
                                for g8 in range(2):
                                    for cc in range(8):
                                        c = g8 * 8 + cc
                                        S.op('pe', mk('transpose', out=PST[:, cc * 128:(cc + 1) * 128], in_=dstT[:, c * 128:(c + 1) * 128],
                                                      identity=ident_b[:]), r=[dkey, 'identb'], w=['pst'])
                                    if ti < 4:
                                        S.op('act', mk('copy', out=xtm[:, g8 * 8:(g8 + 1) * 8, ti * 128:(ti + 1) * 128],
                                                       in_=PST[:].rearrange("p (k n) -> p k n", k=8)), r=['pst'], w=['xtm'])
                                    else:
                                        S.op('act', mk('copy', out=Btm[:, g8 * 8:(g8 + 1) * 8, :],
                                                       in_=PST[:].rearrange("p (k n) -> p k n", k=8)), r=['pst'], w=['Btm'])

                    S.barrier()
                    with contextlib.ExitStack() as pb:
                        Xw = sb('Xw', [128, 2, 512], BF16, pb)
                        dg = sb('dg', [128, 4, 128], F32, pb)
                        tq = sb('tq', [128, 4, 128], F32, pb)
                        ER = sb('ER', [128, 4, 128], F32, pb)
                        MT = sb('MT', [128, 2, 4, 128], BF16, pb)
                        CsT = sb('CsT', [128, 2, 4, 128], BF16, pb)
                        zt = sb('zt', [128, 512], F32, pb)
                        yg = sb('yg', [128, 512], F32, pb)
                        ygb = sb('ygb', [128, 512], BF16, pb)
                        ygT = sb('ygT', [128, 4, 128], BF16, pb)
                        junk3 = sb('junk3', [128, 512], BF16, pb)
                        sast = [sb('sast%d' % i_, [128, 512], BF16, pb) for i_ in range(2)]
                        sald = [sb('sald%d' % i_, [128, 512], BF16, pb) for i_ in range(2)]
                        def bc8(t3, c, d):
                            return t3[:, c, d * 16 + g * 8:d * 16 + g * 8 + 8].unsqueeze(2).to_broadcast([128, 8, 64])

                        def state_update(c, d):
                            S.op('dve', mk('tensor_tensor', out=Xs[:].rearrange("p (h q) -> p h q", h=8),
                                           in0=xtm[:, c, :].rearrange("p (h q) -> p h q", h=8), in1=bc8(w2, c, d), op=ALU.mult),
                                 r=['xtm', 'w2'], w=['Xs'])
                            ps, pk = bank()
                            S.op('pe', mk('matmul', ps[:, :], lhsT=Btm[:, c, :], rhs=Xs[:], start=True, stop=True), r=['Btm', 'Xs'], w=[pk])
                            S.op('dve', mk('tensor_tensor', out=Sst[:].rearrange("p (h q) -> p h q", h=8),
                                           in0=Sst[:].rearrange("p (h q) -> p h q", h=8), in1=bc8(etot, c, d), op=ALU.mult),
                                 r=['Sst', 'etot'], w=['Sst'])
                            S.op('dve', mk('tensor_tensor', out=Sst[:], in0=Sst[:], in1=ps[:, :], op=ALU.add), r=['Sst', pk], w=['Sst'])

                        S.op('dve', mk('memset', Sst[:], 0.0), w=['Sst'])
                        for c in range(NCH):
                            S.op('act', mk('copy', out=sast[c % 2][:], in_=Sst[:]), r=['Sst'], w=[('sast', c % 2)])
                            S.dma('sp', mk('dma_start', out=sa_d[c], in_=sast[c % 2][:]), 'sast%d' % (c % 2), r=[('sast', c % 2)], w=[('sad', c)])
                            state_update(c, 0)
                        S.dma('sp', mk('dma_start', out=ss_ccin, in_=Sst[:]), 'ccs', r=['Sst'], w=['ccin0'])
                        S.dma('pool', mk('collective_compute', "AllGather", ALU.bypass, replica_groups=[[0, 1], [2, 3], [4, 5], [6, 7]],
                                         ins=[ss_ccin], outs=[ss_ccout]), 'cc', r=['ccin0'], w=['ccout0'], inc=1)
                        S.dma('sp', mk('dma_start', out=yg[:], in_=ss_ccout[0:128, :]), 'ccl', r=['ccout0'], w=['yg'])
                        S.dma('sp', mk('dma_start', out=zt[:], in_=ss_ccout[128:256, :]), 'ccl2', r=['ccout0'], w=['zt'])
                        S.op('dve', mk('tensor_scalar_mul', out=Sst[:], in0=yg[:], scalar1=sel[:, 0:1]), r=['yg', 'sel'], w=['Sst'])
                        S.op('dve', mk('scalar_tensor_tensor', out=Sst[:], in0=zt[:], scalar=sel[:, 1:2], in1=Sst[:],
                                       op0=ALU.mult, op1=ALU.add), r=['zt', 'sel', 'Sst'], w=['Sst'])
                        (wz,), wzkey = W.get([('ab_w', (slice(None), slice(g * 512, (g + 1) * 512)), 8, 512)])
                        (wo4,), wokey = W.get([('ab_wo', (slice(g * 512, (g + 1) * 512), slice(None)), 4, D)], free_prev=False)
                        for c in range(NCH - 1, -1, -1):
                            S.op('act', mk('copy', out=Sbf[:], in_=Sst[:]), r=['Sst'], w=['Sbf'])
                            S.dma('sp', mk('dma_start', out=sald[c % 2][:], in_=sa_d[c]), 'sald%d' % (c % 2), r=[('sad', c)], w=[('sald', c % 2)])
                            S.op('pe', mk('matmul', P4[:, 384:512], lhsT=BT[:, c * 128:(c + 1) * 128], rhs=CT[:, c * 128:(c + 1) * 128],
                                          start=True, stop=True), r=['BT', 'CT'], w=[('p4', 3)])
                            for d in range(2):
                                S.op('dve', mk('tensor_tensor', out=Xw[:, d, :].rearrange("p (h q) -> p h q", h=8),
                                               in0=xtm[:, c, :].rearrange("p (h q) -> p h q", h=8), in1=bc8(dt, c, d), op=ALU.mult),
                                     r=['xtm', 'dt'], w=['Xw'])
                            for hq in range(2):
                                for d in range(2):
                                    col0 = d * 16 + g * 8 + hq * 4
                                    ps, pk = bank()
                                    for i in range(4):
                                        S.op('dve', mk('tensor_scalar_mul', out=dg[:, i, :], in0=identf, scalar1=cs[:, c, col0 + i:col0 + i + 1]),
                                             r=['cs', 'cst'], w=[('dg', i)])
                                        S.op('pe', mk('matmul', ps[:, i * 128:(i + 1) * 128], lhsT=ones_f, rhs=dg[:, i, :], start=True, stop=True),
                                             r=[('dg', i), 'cst'], w=[pk])
                                    msk = mneg if d == 0 else mnegT
                                    S.op('dve', mk('tensor_tensor', out=tq[:], in0=ps[:, :].rearrange("p (i t) -> p i t", i=4),
                                                   in1=msk.unsqueeze(1).to_broadcast([128, 4, 128]), op=ALU.add), r=[pk, 'cst'], w=['tq'])
                                    S.op('dve', mk('tensor_tensor', out=tq[:], in0=tq[:],
                                                   in1=ncs[:, c, col0:col0 + 4].unsqueeze(2).to_broadcast([128, 4, 128]), op=ALU.add),
                                         r=['tq', 'ncs'], w=['tq'])
                                    S.op('act', mk('activation', out=tq[:], in_=tq[:], func=AF.Exp), r=['tq'], w=['tq'])
                                    S.op('act', mk('activation', out=ER[:], in_=ps[:, :].rearrange("p (i t) -> p i t", i=4), func=AF.Exp),
                                         r=[pk], w=['ER'])
                                    S.op('dve', mk('tensor_tensor', out=MT[:, d, :, :], in0=tq[:],
                                                   in1=P4[:, 384:512].unsqueeze(1).to_broadcast([128, 4, 128]), op=ALU.mult),
                                         r=['tq', ('p4', 3)], w=[('MT', d)])
                                    S.op('dve', mk('tensor_tensor', out=CsT[:, d, :, :], in0=ER[:],
                                                   in1=CT[:, c * 128:(c + 1) * 128].unsqueeze(1).to_broadcast([128, 4, 128]), op=ALU.mult),
                                         r=['ER', 'CT'], w=[('CsT', d)])
                                psy, pky = bank()
                                for i in range(4):
                                    hh = hq * 4 + i
                                    ysl = psy[:, i * 64:(i + 1) * 64]
                                    S.op('pe', mk('matmul', ysl, lhsT=MT[:, 0, i, :], rhs=Xw[:, 0, hh * 64:(hh + 1) * 64], start=True, stop=False),
                                         r=[('MT', 0), 'Xw'], w=[pky])
                                    S.op('pe', mk('matmul', ysl, lhsT=MT[:, 1, i, :], rhs=Xw[:, 1, hh * 64:(hh + 1) * 64], start=False, stop=False),
                                         r=[('MT', 1), 'Xw'], w=[pky])
                                    S.op('pe', mk('matmul', ysl, lhsT=CsT[:, 0, i, :], rhs=sald[c % 2][:, hh * 64:(hh + 1) * 64], start=False, stop=False),
                                         r=[('CsT', 0), ('sald', c % 2)], w=[pky])
                                    S.op('pe', mk('matmul', ysl, lhsT=CsT[:, 1, i, :], rhs=Sbf[:, hh * 64:(hh + 1) * 64], start=False, stop=True),
                                         r=[('CsT', 1), 'Sbf'], w=[pky])
                                hs = slice(hq * 256, (hq + 1) * 256)
                                S.op('dve', mk('tensor_tensor', out=yg[:, hs].rearrange("p (h q) -> p h q", h=4),
                                               in0=xtm[:, c, hs].rearrange("p (h q) -> p h q", h=4),
                                               in1=dsk[:, g * 8 + hq * 4:g * 8 + hq * 4 + 4].unsqueeze(2).to_broadcast([128, 4, 64]), op=ALU.mult),
                                     r=['xtm', 'ab_dsk'], w=['yg'])
                                S.op('dve', mk('tensor_tensor', out=yg[:, hs], in0=yg[:, hs], in1=psy[:, 0:256], op=ALU.add),
                                     r=['yg', pky], w=['yg'])
                            ps, pk = bank()
                            for kt in range(8):
                                S.op('pe', mk('matmul', ps[:, :], lhsT=hT[:, kt, c * 128:(c + 1) * 128], rhs=wz[:, kt, :],
                                              start=(kt == 0), stop=(kt == 7)), r=[hk[c], wzkey], w=[pk])
                            S.op('act', mk('activation', out=zt[:], in_=ps[:, :], func=AF.Silu), r=[pk], w=['zt'])
                            S.op('dve', mk('tensor_tensor', out=yg[:], in0=yg[:], in1=zt[:], op=ALU.mult), r=['yg', 'zt'], w=['yg'])
                            S.op('act', mk('activation', out=junk3[:], in_=yg[:], func=AF.Square, accum_out=ssq2[:, g, c:c + 1]),
                                 r=['yg', 'ssq2'], w=['junk3', 'ssq2'])
                            S.op('dve', mk('tensor_copy', out=ygb[:], in_=yg[:]), r=['yg'], w=['ygb'])
                            for t in range(4):
                                S.op('pe', mk('transpose', out=PST[:, t * 128:(t + 1) * 128], in_=ygb[:, t * 128:(t + 1) * 128], identity=ident_b[:]),
                                     r=['ygb', 'identb'], w=['pst'])
                            for t in range(4):
                                S.op('act', mk('mul', out=ygT[:, t, :], in_=PST[:, t * 128:(t + 1) * 128], mul=snc[:, g * 4 + t:g * 4 + t + 1]),
                                     r=['pst', 'ab_sn'], w=['ygT'])
                            acc_mm(c, [(ygT[:, t, :], ['ygT']) for t in range(4)], [wo4[:, t, :] for t in range(4)], wokey, first=(g == 0))
                            state_update(c, 1)
                    S.barrier()
                rs = sb('rs', [128, NCH], F32, pm)
                S.op('dve', mk('tensor_tensor', out=rs[:], in0=ssq2[:, 0, :], in1=ssq2[:, 1, :], op=ALU.add), r=['ssq2'], w=['rs'])
                S.op('act', mk('activation', out=rs[:], in_=rs[:], func=AF.Sqrt, bias=epsc[:], scale=1.0 / 1024), r=['rs', 'epsc'], w=['rs'])
                S.op('dve', mk('reciprocal', out=rs[:], in_=rs[:]), r=['rs'], w=['rs'])
                for c in range(NCH):
                    S.op('dve', mk('tensor_scalar_mul', out=acc[:, c, :], in0=acc[:, c, :], scalar1=rs[:, c:c + 1]),
                         r=[('acc', c), 'rs'], w=[('acc', c)])
            S.barrier()

        def na():
            hk = [('hT', c) for c in range(NCHX)]
            with contextlib.ExitStack() as pm:
                qT = sb('nqT', [128, NTOK], BF16, pm)
                kT = sb('nkT', [128, NEXT], BF16, pm)
                vtm = sb('vtm', [128, NCHX, 2, 66], BF16, pm)
                bt = sb('bt', [128, 2, 3, 640], F32, pm)
                sbt = sb('sbt', [128, 640], F32, pm)
                PT = sb('PT', [128, 5, 128], BF16, pm)
                rsn = sb('rsn', [128, 1], F32, pm)
                yna = sb('yna', [128, 128], BF16, pm)
                ynT = sb('ynT', [128, 128], BF16, pm)
                S.op('dve', mk('memset', vtm[:, :, :, 64:66], 1.0), w=['vtm'])
                for j in range(8):
                    (wq, wk_, wv, wo1), wkey = W.get([
                        ('ab_w', (slice(None), slice(2592 + j * 128, 2592 + (j + 1) * 128)), 8, 128),
                        ('ab_w', (slice(None), slice(3616 + j * 128, 3616 + (j + 1) * 128)), 8, 128),
                        ('ab_w', (slice(None), slice(4640 + j * 128, 4640 + (j + 1) * 128)), 8, 128),
                        ('ab_wo', (slice(1024 + j * 128, 1024 + (j + 1) * 128), slice(None)), 1, D)])
                    S.dma('sp', mk('dma_start', out=bt[:], in_=I['ab_bias'][2 * j:2 * j + 2].rearrange("h v p n -> p h v n")),
                          'd_bt', w=['bt'])
                    for (wsrc, dstT, scl, ntb, dk_) in ((wq, qT, 0.125, 4, 'nqT'), (wk_, kT, 1.0, 5, 'nkT')):
                        for tb in range(ntb):
                            n0, n1 = tb * 512, min((tb + 1) * 512, NEXT)
                            ps, pk = bank()
                            for kt in range(8):
                                S.op('pe', mk('matmul', ps[:, 0:n1 - n0], lhsT=wsrc[:, kt, :], rhs=hT[:, kt, n0:n1],
                                              start=(kt == 0), stop=(kt == 7)), r=hk[tb * 4:min(tb * 4 + 4, NCHX)] + [wkey], w=[pk])
                            S.op('act', mk('mul', out=dstT[:, n0:n1], in_=ps[:, 0:n1 - n0], mul=scl), r=[pk], w=[dk_])
                    for c in range(NCHX):
                        ps, pk = bank()
                        for kt in range(8):
                            S.op('pe', mk('matmul', ps[:, 0:128], lhsT=hT[:, kt, c * 128:(c + 1) * 128], rhs=wv[:, kt, :],
                                          start=(kt == 0), stop=(kt == 7)), r=[hk[c], wkey], w=[pk])
                        S.op('act', mk('copy', out=vtm[:, c, :, 0:64], in_=ps[:, 0:128].rearrange("p (e q) -> p e q", e=2)),
                             r=[pk], w=['vtm'])
                    for m in range(NCH):
                        j0 = min(max(m - 2, 0), 13)
                        var = min(m, 2)
                        for e_ in range(2):
                            prt = slice(e_ * 64, (e_ + 1) * 64)
                            for kt in range(5):
                                S.op('pe', mk('matmul', PS2[:, kt * 128:(kt + 1) * 128], lhsT=kT[prt, (j0 + kt) * 128:(j0 + kt + 1) * 128],
                                              rhs=qT[prt, m * 128:(m + 1) * 128], start=True, stop=True), r=['nkT', 'nqT'], w=['ps2'])
                            S.op('dve', mk('tensor_tensor', out=sbt[:, 0:512], in0=PS2[:, 0:512], in1=bt[:, e_, var, 0:512], op=ALU.add),
                                 r=['ps2', 'bt'], w=['sbt'])
                            S.op('dve', mk('tensor_tensor', out=sbt[:, 512:640], in0=PS2[:, 512:640], in1=bt[:, e_, var, 512:640], op=ALU.add),
                                 r=['ps2', 'bt', 'sbt'], w=['sbt'])
                            S.op('act', mk('activation', out=PT[:].rearrange("p k n -> p (k n)"), in_=sbt[:], func=AF.Exp), r=['sbt'], w=['PT'])
                            ps, pk = bank()
                            for kt in range(5):
                                S.op('pe', mk('matmul', ps[:, 0:65], lhsT=PT[:, kt, :], rhs=vtm[:, j0 + kt, e_, 0:65],
                                              start=(kt == 0), stop=(kt == 4)), r=['PT', 'vtm'], w=[pk])
                            S.op('dve', mk('reciprocal', out=rsn[:], in_=ps[:, 64:65]), r=[pk], w=['rsn'])
                            S.op('dve', mk('tensor_scalar_mul', out=yna[:, prt], in0=ps[:, 0:64], scalar1=rsn[:, 0:1]),
                                 r=[pk, 'rsn'], w=['yna'])
                        S.op('pe', mk('transpose', out=PST[:, 0:128], in_=yna[:], identity=ident_b[:]), r=['yna', 'identb'], w=['pst'])
                        S.op('act', mk('copy', out=ynT[:], in_=PST[:, 0:128]), r=['pst'], w=['ynT'])
                        acc_mm(m, [(ynT[:], ['ynT'])], [wo1[:, 0, :]], wkey, first=False)
            S.barrier()

        def body():
            if stage in ('mlp_only', 'l1'):
                zero_acc()
            else:
                if stage == 'na':
                    zero_acc()
                else:
                    ssd()
                if stage != 'ssd':
                    na()
            if stage in ('ssd', 'na', 'l0'):
                run_epilogue(0, 0, I['x'], 'xin', out, None)
                return
            run_epilogue(0, 0, I['x'], 'xin', xs, (0, (4, 3)))
            mlp(0)
            run_epilogue(0, 1, xs, 'xs', xs, (1, (1, 0)))
            if stage == 'mlp_only':
                zero_acc()
            else:
                mlstm()
            run_epilogue(1, 0, xs, 'xs', xs, (1, (4, 3)))
            mlp(1)
            run_epilogue(1, 1, xs, 'xs', out, None)
        body()

        if dry:
            return W.rec
        with nc.Block() as block:
            S.replay(block)
    return None


def _consts():
    s = np.arange(128)[:, None]
    t = np.arange(128)[None, :]
    c = np.zeros((128, 6, 128), np.float32)
    c[:, 0] = (s == t)
    c[:, 1] = (s <= t)
    c[:, 2] = (s >= t)
    c[:, 3] = np.where(s > t, NEG, 0.0)
    c[:, 4] = np.where(s < t, NEG, 0.0)
    c[:, 5] = 1.0
    return c


def _na_bias_tables(rpb, flipped):
    out = np.full((16, 3, 128, 5, 128), NEG, np.float32)
    kp = np.arange(128); qp = np.arange(128)
    for var in range(3):
        m = var
        j0 = 0
        for kt in range(5):
            lkr = 2 * (j0 + kt) + kp // 64
            lkc = kp % 64
            lqr = 2 * m + qp // 64
            lqc = qp % 64
            if flipped:
                gkr, gkc, gqr, gqc = 63 - lkr, 63 - lkc, 63 - lqr, 63 - lqc
            else:
                gkr, gkc, gqr, gqc = lkr, lkc, lqr, lqc
            rs = np.clip(gqr - 4, 0, 56); cs_ = np.clip(gqc - 8, 0, 48)
            dr = gkr[:, None] - gqr[None, :]
            dc = gkc[:, None] - gqc[None, :]
            valid = ((gkr[:, None] >= rs[None, :]) & (gkr[:, None] < rs[None, :] + 8) &
                     (gkc[:, None] >= cs_[None, :]) & (gkc[:, None] < cs_[None, :] + 16) &
                     (gkr[:, None] >= 0) & (gkr[:, None] < 64))
            ro = np.clip(dr + 7, 0, 14); co = np.clip(dc + 15, 0, 30)
            vals = rpb[:, ro, co]
            out[:, var, :, kt, :] = np.where(valid[None], vals, NEG)
    return np.ascontiguousarray(out.reshape(16, 3, 128, 640))


def _prep_inputs(inp):
    f = lambda k: np.ascontiguousarray(np.asarray(inp[k], dtype=np.float32))
    x = f('x'); c = f('c')
    ada_w = f('ada_w'); ada_b = f('ada_b'); norm_g = f('norm_g')
    shared = {
        'ada_w': ada_w,
        'ada_b': np.ascontiguousarray(np.broadcast_to(ada_b[:, None, :], (2, 128, 6 * D))),
        'ngb': np.ascontiguousarray(np.broadcast_to(norm_g[:, :, None, :], (2, 4, 128, D))),
        'mlp_w1': f('mlp_w1'), 'mlp_w2': f('mlp_w2'),
        'ml_w': f('ml_w_in')[0], 'ml_wo': f('ml_w_out')[0],
        'ml_hn': np.ascontiguousarray(np.broadcast_to(f('ml_head_norm')[0][None, :], (128, 2048))),
        'cst_in': _consts(),
        'ab_w': f('ab_w_in')[0], 'ab_wo': f('ab_w_out')[0],
        'ab_dsk': np.ascontiguousarray(np.broadcast_to(f('ab_d_skip')[0][None, :], (128, 16))),
        'ab_sn': np.ascontiguousarray(f('ab_ssd_norm')[0].reshape(8, 128).T),
        'ab_cb': np.ascontiguousarray(f('ab_conv_b')[0].reshape(12, 128).T),
    }
    rpb = f('ab_rpb')[0]
    btab = [_na_bias_tables(rpb, False), _na_bias_tables(rpb, True)]
    maps = []
    for core in range(8):
        b, s = core // 2, core % 2
        xl = x[b] if s == 0 else x[b][::-1]
        m = dict(shared)
        m['x'] = np.ascontiguousarray(xl[:NEXT])
        m['cvec'] = np.ascontiguousarray(c[b].reshape(8, 128).T)
        gb = f('ml_gate_b')[0]
        if s == 1:
            gb = gb[[2, 3, 0, 1]]
        m['ml_gb'] = np.ascontiguousarray(np.broadcast_to(gb.reshape(1, 32), (128, 32)))
        wgc = f('ml_w_in')[0][:, 6144:6176].reshape(D, 4, 8)
        if s == 1:
            wgc = wgc[:, [2, 3, 0, 1]]
        m['ml_wg'] = np.ascontiguousarray(wgc.reshape(D, 32))
        wdt = f('ab_w_in')[0][:, 2560:2592].reshape(D, 2, 16)
        dtb = f('ab_dt_bias')[0]; alog = f('ab_a_log')[0]; cwv = f('ab_conv_w')[0]
        if s == 1:
            wdt = wdt[:, ::-1]; dtb = dtb[::-1]; alog = alog[::-1]; cwv = cwv[::-1]
        m['ab_wdt'] = np.ascontiguousarray(wdt.reshape(D, 32))
        m['ab_dtb'] = np.ascontiguousarray(np.broadcast_to(dtb.reshape(1, 32), (128, 32)))
        m['ab_alog'] = np.ascontiguousarray(np.broadcast_to(alog.reshape(1, 32), (128, 32)))
        m['ab_cw'] = np.ascontiguousarray(cwv.T.reshape(12, 128, 5).transpose(1, 0, 2))
        m['ab_bias'] = btab[s]
        sel = np.zeros((128, 2), np.float32)
        sel[:, 1 - s] = 1.0
        m['sel'] = sel
        maps.append(m)
    return maps


_NC_CACHE = {}


def kernel(**inputs):
    stage = DEBUG_STAGE
    if stage not in _NC_CACHE:
        _NC_CACHE[stage] = build_program(stage)
    nc = _NC_CACHE[stage]
    maps = _prep_inputs(inputs)
    res = run_bass_kernel_spmd(nc, maps, core_ids=list(range(8)))
    outp = np.empty((4, 4096, D), np.float32)
    for core in range(8):
        b, s = core // 2, core % 2
        o = res.results[core]['out']
        if s == 0:
            outp[b, :NTOK] = o
        else:
            outp[b, NTOK:] = o[::-1]
    return outp
```
